# Optimizing a Trainium2 kernel written in Bass

```python
import math
import jax, jax.numpy as jnp
from jax import lax
import numpy as np

D_MODEL = 1024
BATCH = 16
SEQ = 2048
DEPTH = 1

N_META = 16
GRID_W = 64
Q_BLOCK = 128
ROPE_THETA = 10000.0

MLA_HEADS = 4
Q_LORA = 256
KV_LORA = 128
NOPE_DIM = 128
ROPE_DIM = 64
V_DIM = 128
QK_DIM = NOPE_DIM + ROPE_DIM

GQA_HEADS = 4
GQA_KV_HEADS = 2
GQA_DIM = 128
AXIAL_HALF = GQA_DIM // 2

MLA_WIDTH = MLA_HEADS * V_DIM
GQA_WIDTH = GQA_HEADS * GQA_DIM
MIX_WIDTH = MLA_WIDTH + GQA_WIDTH

IN_SPLITS = (Q_LORA, KV_LORA, ROPE_DIM, GQA_HEADS * GQA_DIM, GQA_KV_HEADS * GQA_DIM, GQA_KV_HEADS * GQA_DIM)
IN_COLS = sum(IN_SPLITS)
IN_OFFSETS = tuple(int(o) for o in np.cumsum(IN_SPLITS)[:-1])

N_EXPERTS = 32
TOP_K = 4
D_FF = D_MODEL
SWIGLU_LIMIT = 7.0
SWIGLU_ALPHA = 1.702

RMS_EPS = 1e-6
LN_EPS = 1e-5
DEEPNORM_ALPHA = (2.0 * DEPTH) ** 0.25
DEEPNORM_BETA = (8.0 * DEPTH) ** -0.25

kernel_name = 'hymba_mla_axialgqa_moe_deepnorm_encoder'


def _rmsnorm(x, g):
    xf = x.astype(jnp.float32)
    y = xf * lax.rsqrt(jnp.mean(xf * xf, axis=-1, keepdims=True) + RMS_EPS)
    return (y * g.astype(jnp.float32)).astype(x.dtype)


def _layernorm(x, g, b):
    xf = x.astype(jnp.float32)
    mu = jnp.mean(xf, axis=-1, keepdims=True)
    var = jnp.mean(jnp.square(xf - mu), axis=-1, keepdims=True)
    y = (xf - mu) * lax.rsqrt(var + LN_EPS)
    return (y * g.astype(jnp.float32) + b.astype(jnp.float32)).astype(x.dtype)


def _rope_cos_sin(pos, dim):
    inv = ROPE_THETA ** (-jnp.arange(0, dim, 2, dtype=jnp.float32) / dim)
    ang = pos.astype(jnp.float32)[:, None] * inv[None, :]
    return jnp.cos(ang)[:, None, :], jnp.sin(ang)[:, None, :]


def _rotate(x, cos, sin):
    xf = x.astype(jnp.float32)
    x1, x2 = jnp.split(xf, 2, axis=-1)
    return jnp.concatenate([x1 * cos - x2 * sin, x1 * sin + x2 * cos], axis=-1).astype(x.dtype)


def _attend(q, k, v, scale):
    s = jnp.einsum('bqkgd,bskd->bkgqs', q, k).astype(jnp.float32) * scale
    p = jax.nn.softmax(s, axis=-1).astype(v.dtype)
    return jnp.einsum('bkgqs,bskd->bqkgd', p, v)


def _blocked_attention(q, k, v, scale):
    B, L, H, Dq = q.shape
    Hk = k.shape[2]
    G = H // Hk
    Dv = v.shape[-1]
    q = q.reshape(B, L, Hk, G, Dq)
    o_meta = _attend(q[:, :N_META], k, v, scale)
    n_blk = (L - N_META) // Q_BLOCK
    qb = q[:, N_META:].reshape(B, n_blk, Q_BLOCK, Hk, G, Dq).transpose(1, 0, 2, 3, 4, 5)
    ob = lax.map(lambda qi: _attend(qi, k, v, scale), qb)
    ob = ob.transpose(1, 0, 2, 3, 4, 5).reshape(B, L - N_META, Hk, G, Dv)
    return jnp.concatenate([o_meta, ob], axis=1).reshape(B, L, H, Dv)


def _mixer(h, pos_1d, row, col, w_in, g_q_a, w_q_b, g_kv_a, w_kv_b, g_q_gqa, g_k_gqa, g_o_mla, g_o_gqa, w_o):
    B, L, _ = h.shape
    z = jnp.einsum('bld,dc->blc', h, w_in)
    q_a, kv_a, k_pe, q_g, k_g, v_g = jnp.split(z, IN_OFFSETS, axis=-1)

    cos1, sin1 = _rope_cos_sin(pos_1d, ROPE_DIM)
    q = jnp.einsum('blr,rc->blc', _rmsnorm(q_a, g_q_a), w_q_b).reshape(B, L, MLA_HEADS, QK_DIM)
    q_m = jnp.concatenate([q[..., :NOPE_DIM], _rotate(q[..., NOPE_DIM:], cos1, sin1)], axis=-1)
    kv = jnp.einsum('blr,rc->blc', _rmsnorm(kv_a, g_kv_a), w_kv_b).reshape(B, L, MLA_HEADS, NOPE_DIM + V_DIM)
    k_rot = _rotate(k_pe[:, :, None, :], cos1, sin1)
    k_m = jnp.concatenate([kv[..., :NOPE_DIM], jnp.broadcast_to(k_rot, (B, L, MLA_HEADS, ROPE_DIM))], axis=-1)
    v_m = kv[..., NOPE_DIM:]
    o_mla = _blocked_attention(q_m, k_m, v_m, QK_DIM ** -0.5).reshape(B, L, MLA_WIDTH)

    cos_r, sin_r = _rope_cos_sin(row, AXIAL_HALF)
    cos_c, sin_c = _rope_cos_sin(col, AXIAL_HALF)

    def axial(t):
        return jnp.concatenate([_rotate(t[..., :AXIAL_HALF], cos_r, sin_r),
                                _rotate(t[..., AXIAL_HALF:], cos_c, sin_c)], axis=-1)

    qg = axial(_rmsnorm(q_g.reshape(B, L, GQA_HEADS, GQA_DIM), g_q_gqa))
    kg = axial(_rmsnorm(k_g.reshape(B, L, GQA_KV_HEADS, GQA_DIM), g_k_gqa))
    vg = v_g.reshape(B, L, GQA_KV_HEADS, GQA_DIM)
    o_gqa = _blocked_attention(qg, kg, vg, GQA_DIM ** -0.5).reshape(B, L, GQA_WIDTH)

    o = jnp.concatenate([_rmsnorm(o_mla, g_o_mla), _rmsnorm(o_gqa, g_o_gqa)], axis=-1)
    return jnp.einsum('blc,cd->bld', o, w_o)


def _clamped_swiglu(hgu):
    x_glu = jnp.minimum(hgu[..., ::2], SWIGLU_LIMIT)
    x_lin = jnp.clip(hgu[..., 1::2], -SWIGLU_LIMIT, SWIGLU_LIMIT)
    return x_glu * jax.nn.sigmoid(SWIGLU_ALPHA * x_glu) * (x_lin + 1.0)


def _moe_sequence(h, w_router, b_router, w_gate_up, b_gate_up, w_down, b_down):
    L = h.shape[0]
    logits = (jnp.einsum('ld,de->le', h, w_router) + b_router).astype(jnp.float32)
    top_val, top_idx = lax.top_k(logits, TOP_K)
    gates = jax.nn.softmax(top_val, axis=-1).astype(h.dtype)
    flat_e = top_idx.reshape(-1)
    order = jnp.argsort(flat_e)
    tok = order // TOP_K
    e_sorted = flat_e[order]
    group_sizes = jnp.bincount(flat_e, length=N_EXPERTS).astype(jnp.int32)
    xs = h[tok]
    hgu = lax.ragged_dot(xs, w_gate_up, group_sizes) + b_gate_up[e_sorted]
    act = _clamped_swiglu(hgu)
    out = lax.ragged_dot(act, w_down, group_sizes) + b_down[e_sorted]
    out = out * gates.reshape(-1)[order][:, None]
    return jax.ops.segment_sum(out, tok, num_segments=L)


def setup_inputs(seed: int = 0) -> dict:
    key = jax.random.key(seed)
    ks = jax.random.split(key, 28)
    f32 = jnp.float32

    def nrm(k, shape, scale):
        return jax.random.normal(k, shape, f32) * scale

    def gain(k, shape):
        return 1.0 + 0.05 * jax.random.normal(k, shape, f32)

    Dp = DEPTH
    return {
        'x': jax.random.normal(ks[0], (BATCH, SEQ, D_MODEL), f32),
        'meta_tokens': nrm(ks[1], (N_META, D_MODEL), 1.0),
        'ln_emb_g': gain(ks[2], (D_MODEL,)),
        'ln_emb_b': nrm(ks[3], (D_MODEL,), 0.02),
        'w_in': nrm(ks[4], (Dp, D_MODEL, IN_COLS), D_MODEL ** -0.5),
        'g_q_a': gain(ks[5], (Dp, Q_LORA)),
        'w_q_b': nrm(ks[6], (Dp, Q_LORA, MLA_HEADS * QK_DIM), Q_LORA ** -0.5),
        'g_kv_a': gain(ks[7], (Dp, KV_LORA)),
        'w_kv_b': nrm(ks[8], (Dp, KV_LORA, MLA_HEADS * (NOPE_DIM + V_DIM)), KV_LORA ** -0.5),
        'g_q_gqa': gain(ks[9], (Dp, GQA_DIM)),
        'g_k_gqa': gain(ks[10], (Dp, GQA_DIM)),
        'g_o_mla': gain(ks[11], (Dp, MLA_WIDTH)),
        'g_o_gqa': gain(ks[12], (Dp, GQA_WIDTH)),
        'w_o': nrm(ks[13], (Dp, MIX_WIDTH, D_MODEL), DEEPNORM_BETA * MIX_WIDTH ** -0.5),
        'ln1_g': gain(ks[14], (Dp, D_MODEL)),
        'ln1_b': nrm(ks[15], (Dp, D_MODEL), 0.02),
        'w_router': nrm(ks[16], (Dp, D_MODEL, N_EXPERTS), D_MODEL ** -0.5),
        'b_router': nrm(ks[17], (Dp, N_EXPERTS), 0.01),
        'w_gate_up': nrm(ks[18], (Dp, N_EXPERTS, D_MODEL, 2 * D_FF), D_MODEL ** -0.5),
        'b_gate_up': nrm(ks[19], (Dp, N_EXPERTS, 2 * D_FF), 0.02),
        'w_down': nrm(ks[20], (Dp, N_EXPERTS, D_FF, D_MODEL), DEEPNORM_BETA * D_FF ** -0.5),
        'b_down': nrm(ks[21], (Dp, N_EXPERTS, D_MODEL), 0.02),
        'ln2_g': gain(ks[22], (Dp, D_MODEL)),
        'ln2_b': nrm(ks[23], (Dp, D_MODEL), 0.02),
    }


def reference(x, meta_tokens, ln_emb_g, ln_emb_b, w_in, g_q_a, w_q_b, g_kv_a, w_kv_b,
              g_q_gqa, g_k_gqa, g_o_mla, g_o_gqa, w_o, ln1_g, ln1_b,
              w_router, b_router, w_gate_up, b_gate_up, w_down, b_down, ln2_g, ln2_b):
    B, S, D = x.shape
    meta = jnp.broadcast_to(meta_tokens.astype(x.dtype)[None], (B, N_META, D))
    h = _layernorm(jnp.concatenate([meta, x], axis=1), ln_emb_g, ln_emb_b)
    L = S + N_META

    rows = S // GRID_W
    pos_1d = jnp.arange(L, dtype=jnp.int32)
    row = jnp.concatenate([jnp.full((N_META,), -1, jnp.int32), jnp.repeat(jnp.arange(rows, dtype=jnp.int32), GRID_W)])
    col = jnp.concatenate([jnp.arange(N_META, dtype=jnp.int32), jnp.tile(jnp.arange(GRID_W, dtype=jnp.int32), rows)])

    for i in range(DEPTH):
        mix = _mixer(h, pos_1d, row, col, w_in[i], g_q_a[i], w_q_b[i], g_kv_a[i], w_kv_b[i],
                     g_q_gqa[i], g_k_gqa[i], g_o_mla[i], g_o_gqa[i], w_o[i])
        h = _layernorm(DEEPNORM_ALPHA * h + mix, ln1_g[i], ln1_b[i])
        moe = lax.map(lambda hs: _moe_sequence(hs, w_router[i], b_router[i], w_gate_up[i],
                                               b_gate_up[i], w_down[i], b_down[i]), h)
        h = _layernorm(DEEPNORM_ALPHA * h + moe, ln2_g[i], ln2_b[i])

    return h[:, N_META:]
```

```python
import numpy as np
from contextlib import ExitStack
import concourse.bass as bass
import concourse.mybir as mybir
from concourse.bass_utils import run_bass_kernel_spmd

F32 = mybir.dt.float32
BF16 = mybir.dt.bfloat16
I32 = mybir.dt.int32
U32 = mybir.dt.uint32
AF = mybir.ActivationFunctionType
ALU = mybir.AluOpType
AX = mybir.AxisListType

NCORES = 8
SEQ = 2048
NMETA = 16
L = SEQ + NMETA
D = 1024
NE = 32
CAP = 1024
NSLOT = NE * CAP
BIG = float(1 << 20)
ALPHA = 2.0 ** 0.25
RMS_EPS = 1e-6
LN_EPS = 1e-5
CH = 4000

P_, A_, V_, G_, Q_ = "pe", "act", "dve", "pool", "sp"


import os
_STOP = os.environ.get("K_STOP", "")
_SMALL = _STOP in ("s0", "1a", "1b", "2a")
_TLIM = int(os.environ.get("K_TLIM", "17"))
_SUB = os.environ.get("K_SUB", "")


class _Stop(Exception):
    pass


class Buf:
    def __init__(self, name):
        self.name = name
        self.w = None
        self.r = {}


class DSem:
    def __init__(self, h):
        self.h = h
        self.count = 0


class Rec:
    def __init__(self, nc, stack):
        self.nc = nc
        self.stack = stack
        self.prog = {e: [] for e in (P_, A_, V_, G_, Q_)}
        self.cnt = {e: 0 for e in self.prog}
        self.esem = {e: [] for e in self.prog}
        self.seen = {e: {} for e in self.prog}
        self.nsem = 0
        self.dsems = []
        self.free_dsems = {}
        self._cur = None
        self._streams = {}

    def new_sem(self, name):
        self.nsem += 1
        return self.stack.enter_context(self.nc.semaphore(name))

    def dsem(self, name, stack=None, kind="hw"):
        pool = self.free_dsems.setdefault(kind, [])
        if pool:
            d = pool.pop()
        else:
            d = DSem(self.new_sem(name))
            self.dsems.append(d)
        if stack is not None:
            stack.callback(lambda d=d, pool=pool: pool.append(d))
        return d

    def flush(self):
        self.barrier()
        with self.nc.Block() as blk:
            @blk.sync
            def _(e):
                for f in self.prog[Q_]:
                    f(e)

            @blk.tensor
            def _(e):
                for f in self.prog[P_]:
                    f(e)

            @blk.scalar
            def _(e):
                for f in self.prog[A_]:
                    f(e)

            @blk.vector
            def _(e):
                for f in self.prog[V_]:
                    f(e)

            @blk.gpsimd
            def _(e):
                self.bc_reg = None
                for f in self.prog[G_]:
                    f(e)
        for e in self.prog:
            self.prog[e] = []

    def bc(self, e):
        if self.bc_reg is None:
            self.bc_reg = e.to_reg(NSLOT - 1)
        return self.bc_reg

    def barrier(self):
        toks = []
        for e2 in self.prog:
            if self.cnt[e2] > 0:
                i = self.cnt[e2] - 1
                toks.append((self.esem[e2][i // CH], i % CH + 1, e2, False))
        for d in self.dsems:
            if d.count > 0:
                toks.append((d.h, d.count, None, True))
        for e in self.prog:
            for t in toks:
                if t[2] == e and not t[3]:
                    continue
                self._wait(e, t)

    def _etoken(self, e):
        i = self.cnt[e]
        k = i // CH
        while len(self.esem[e]) <= k:
            self.esem[e].append(self.new_sem(f"e_{e}{len(self.esem[e])}"))
        self.cnt[e] += 1
        return (self.esem[e][k], i % CH + 1, e, False)

    def _wait(self, e, tok):
        sem, val = tok[0], tok[1]
        if self.seen[e].get(sem, 0) >= val:
            return
        self.seen[e][sem] = val
        self.prog[e].append(lambda eng, sem=sem, val=val: eng.wait_ge(sem, val))

    def defer(self, name):
        rec = self

        class _D:
            def __enter__(s_):
                rec._cur = rec._streams.setdefault(name, [])

            def __exit__(s_, *a):
                rec._cur = None
                return False
        return _D()

    def interleave(self):
        lists = [l for l in self._streams.values() if l]
        self._streams = {}
        idx = [0] * len(lists)
        alive = True
        while alive:
            alive = False
            for i, l in enumerate(lists):
                if idx[i] < len(l):
                    self.op(*l[idx[i]])
                    idx[i] += 1
                    alive = True

    def op(self, e, fns, reads=(), writes=(), dsem=None):
        if self._cur is not None:
            self._cur.append((e, fns, list(reads), list(writes), dsem))
            return None
        if callable(fns):
            fns = [fns]
        for b in reads:
            if b.w is not None:
                self._wait(e, b.w)
            if getattr(b, "psum", False):
                for t in b.r.values():
                    if t[2] != e:
                        self._wait(e, t)
        for b in writes:
            if b.w is not None and (b.w[2] != e or b.w[3] or e != P_):
                self._wait(e, b.w)
            for t in b.r.values():
                if t[2] != e or t[3] or e != P_:
                    self._wait(e, t)
        if dsem is None:
            tok = self._etoken(e)
            inc = 1
        else:
            dsem.count += 16
            tok = (dsem.h, dsem.count, e, True)
            inc = 16
        n = len(fns)
        for i, fn in enumerate(fns):
            if i == n - 1:
                self.prog[e].append(lambda eng, fn=fn, s=tok[0], inc=inc: fn(eng).then_inc(s, inc))
            else:
                self.prog[e].append(lambda eng, fn=fn: fn(eng))
        for b in reads:
            b.r[tok[0]] = tok
        for b in writes:
            b.w = tok
            b.r = {}
        return tok

    def wait_tok(self, e, tok):
        self._wait(e, tok)


class T:
    _n = [0]

    def __init__(self, rec, stack, nc, name, shape, dt, psum=False, dma=False):
        T._n[0] += 1
        name = f"t{T._n[0]}_{name}"
        if psum:
            self.t = stack.enter_context(nc.psum_tensor(name, list(shape), dt))
        else:
            self.t = stack.enter_context(nc.sbuf_tensor(name, list(shape), dt))
        self.b = Buf(name)
        self.b.psum = psum
        self.s = rec.dsem("d_" + name, stack, "sw" if dma == "sw" else "hw") if dma else None

    def __getitem__(self, k):
        return self.t[k]


def build_nc():
    nc = bass.Bass("TRN2", target_bir_lowering=False)

    def din(name, shape, dt=F32):
        return nc.dram_tensor(name, list(shape), dt, kind="ExternalInput").ap()

    x_d = din("x", [2, SEQ, D])
    meta_d = din("meta", [NMETA, D])
    vecs_d = din("vecs", [8, D])
    gsm_d = din("gsm", [2, 128])
    gpp_d = din("gpp", [128, 4])
    win_d = din("w_in", [D, 1472])
    wqb_d = din("w_qb", [256, 768])
    wkvb_d = din("w_kvb", [128, 1024])
    wo_d = din("w_o", [D, D])
    wr_d = din("w_r", [D, NE])
    br_d = din("b_r", [1, NE])
    w1_d = din("w1", [NE if not _SMALL else 1, D, 2048])
    b1_d = din("b1t", [128, NE, 16])
    w2_d = din("w2", [NE if not _SMALL else 1, D, D])
    b2_d = din("b2", [NE, D])
    rope_d = din("rope", [L, 384])
    cst_d = din("cst", [128, 448])
    out_d = nc.dram_tensor("out", [2, SEQ, D], F32, kind="ExternalOutput").ap()
    h0s_d = nc.dram_tensor("h0s", [SEQ, D], F32).ap()
    h1s_d = nc.dram_tensor("h1s", [2 * SEQ, D], F32).ap()
    xs_d = nc.dram_tensor("xs", [NSLOT, D], BF16).ap()
    ys_d = nc.dram_tensor("ys", [NSLOT, D], BF16).ap()

    es = ExitStack()
    try:
      with es:
        rec = Rec(nc, es)

        def mk(stack, name, shape, dt, psum=False, dma=False):
            return T(rec, stack, nc, name, shape, dt, psum=psum, dma=dma)

        def dma(e, out, in_, reads, writes, sem):
            return rec.op(e, lambda eng: eng.dma_start(out=out, in_=in_), reads=reads, writes=writes, dsem=sem)

        def ck(tag):
            if _SUB == tag:
                rec.flush()
                raise _Stop()

        cst = mk(es, "cst", [128, 448], F32, dma=True)
        ident_b = mk(es, "ident_b", [128, 128], BF16)
        U_b = mk(es, "U_b", [128, 128], BF16)
        ones_b = mk(es, "ones_b", [128, 128], BF16)
        gpp = mk(es, "gpp", [128, 4], F32, dma=True)
        gsm = mk(es, "gsm", [128, 2, 128], F32, dma=True)
        eps_ln = mk(es, "eps_ln", [128, 1], F32)
        eps_rms = mk(es, "eps_rms", [128, 1], F32)
        slot_c_all = mk(es, "slot_c_all", [128, 32, 4], I32)
        gates_all = mk(es, "gates_all", [128, 32, 4], F32)
        dram_xs = Buf("xs")
        dram_ys = Buf("ys")
        dram_h0 = [Buf(f"h0s{i}") for i in range(16)]
        dram_h1 = [Buf(f"h1s{i}") for i in range(32)]

        dma(Q_, cst[:, :], cst_d[:, :], [], [cst.b], cst.s)
        dma(Q_, gpp[:, :], gpp_d[:, :], [], [gpp.b], gpp.s)
        dma(Q_, gsm[:, :, :], gsm_d.unsqueeze(0).to_broadcast([128, 2, 128]), [], [gsm.b], gsm.s)
        rec.op(V_, lambda e: e.tensor_copy(out=ident_b[:, :], in_=cst[:, 0:128]), [cst.b], [ident_b.b])
        rec.op(V_, lambda e: e.tensor_copy(out=U_b[:, :], in_=cst[:, 128:256]), [cst.b], [U_b.b])
        rec.op(V_, lambda e: e.tensor_copy(out=ones_b[:, :], in_=cst[:, 256:384]), [cst.b], [ones_b.b])
        rec.op(V_, lambda e: e.memset(eps_ln[:, :], LN_EPS), [], [eps_ln.b])
        rec.op(V_, lambda e: e.memset(eps_rms[:, :], RMS_EPS), [], [eps_rms.b])
        xs_v = xs_d.rearrange("(k p a) d -> k p a d", p=128, a=4)

        zf = {}

        def zero_fill_init(stack):
            zf["b"] = mk(stack, "zero_b", [128, 4096], BF16, dma=True)
            rec.op(G_, lambda e: e.memset(zf["b"][:, :], 0.0), [], [zf["b"].b])

        def zero_fill_chunks(k0, k1):
            zb = zf["b"]
            for k in range(k0, min(k1, NSLOT // 512)):
                dma(Q_, xs_v[k], zb[:, :].rearrange("p (a d) -> p a d", a=4), [zb.b], [dram_xs], zb.s)

        def bcast_row(ap_row, n):
            return ap_row.to_broadcast([128, n])

        def layernorm_g(src, R, g_bc, b_bc, dst, tmp, st, mv, sd, rstd, nmr, gmul=V_):
            rec.op(V_, lambda e: e.bn_stats(out=st[:R, 0:6], in_=src[:R, 0:512]), [src.b], [st.b])
            yield
            rec.op(V_, lambda e: e.bn_stats(out=st[:R, 6:12], in_=src[:R, 512:1024]), [src.b], [st.b])
            yield
            rec.op(V_, lambda e: e.bn_aggr(out=mv[:R, :], in_=st[:R, :]), [st.b], [mv.b])
            yield
            rec.op(A_, lambda e: e.activation(out=sd[:R, :], in_=mv[:R, 1:2], func=AF.Sqrt, bias=eps_ln[:R, :], scale=1.0),
                   [mv.b, eps_ln.b], [sd.b])
            yield
            rec.op(V_, lambda e: e.reciprocal(out=rstd[:R, :], in_=sd[:R, :]), [sd.b], [rstd.b])
            yield
            rec.op(V_, lambda e: e.tensor_scalar(out=nmr[:R, :], in0=mv[:R, 0:1], scalar1=rstd[:R, :], scalar2=-1.0,
                                                  op0=ALU.mult, op1=ALU.mult), [mv.b, rstd.b], [nmr.b])
            yield
            rec.op(A_, lambda e: e.activation(out=tmp[:R, :], in_=src[:R, :], func=AF.Identity, bias=nmr[:R, :], scale=rstd[:R, :]),
                   [src.b, nmr.b, rstd.b], [tmp.b])
            yield
            rec.op(gmul, lambda e: e.tensor_tensor(out=tmp[:R, :], in0=tmp[:R, :], in1=g_bc[:R, :], op=ALU.mult),
                   [tmp.b, g_bc.b], [tmp.b])
            yield
            rec.op(G_, lambda e: e.tensor_tensor(out=dst[:R, :], in0=tmp[:R, :], in1=b_bc[:R, :], op=ALU.add),
                   [tmp.b, b_bc.b], [dst.b])
            yield

        def layernorm(*a, **k):
            for _ in layernorm_g(*a, **k):
                pass

        def pipeline(make_gen, n, nb, stag):
            active = {}
            free = list(range(nb))
            nxt = 0
            rnd = 0
            while nxt < n or active:
                if nxt < n and free and rnd % stag == 0:
                    i = free.pop(0)
                    active[i] = make_gen(nxt, i)
                    nxt += 1
                for i in list(active):
                    try:
                        next(active[i])
                    except StopIteration:
                        del active[i]
                        free.append(i)
                rnd += 1

        def lockstep(gens):
            gens = list(gens)
            while gens:
                for g_ in list(gens):
                    try:
                        next(g_)
                    except StopIteration:
                        gens.remove(g_)

        with ExitStack() as s1:
            w_qb = mk(s1, "w_qb", [128, 2, 768], BF16)
            w_kvb = mk(s1, "w_kvb", [128, 1024], BF16)
            with ExitStack() as s0:
                stg = mk(s0, "stg_qb", [128, 2, 768], F32, dma=True)
                stg2 = mk(s0, "stg_kvb", [128, 1024], F32, dma=True)
                dma(Q_, stg[:, :, :], wqb_d.rearrange("(c p) n -> p c n", p=128), [], [stg.b], stg.s)
                dma(Q_, stg2[:, :], wkvb_d[:, :], [], [stg2.b], stg2.s)
                for c in range(2):
                    rec.op(V_, lambda e, c=c: e.tensor_scalar(out=w_qb[:, c, :], in0=stg[:, c, :], scalar1=gpp[:, c:c + 1],
                                                               scalar2=None, op0=ALU.mult), [stg.b, gpp.b], [w_qb.b])
                rec.op(V_, lambda e: e.tensor_scalar(out=w_kvb[:, :], in0=stg2[:, :], scalar1=gpp[:, 2:3], scalar2=None,
                                                      op0=ALU.mult), [stg2.b, gpp.b], [w_kvb.b])
                rec.flush()
                if _STOP == "s0":
                    raise _Stop()

            qTn = mk(s1, "qTn", [128, 4, SEQ], BF16)
            qTr = mk(s1, "qTr", [128, 2, SEQ], BF16)
            qTg = mk(s1, "qTg", [128, 4, SEQ], BF16)
            kTn = mk(s1, "kTn", [128, 4, L], BF16)
            kTr = mk(s1, "kTr", [128, L], BF16)
            kTg = mk(s1, "kTg", [128, 2, L], BF16)
            vm = mk(s1, "vm", [128, 17, 4, 129], BF16)
            vg = mk(s1, "vg", [128, 17, 2, 129], BF16)
            rec.op(G_, lambda e: e.memset(vm[:, :, :, 128:129], 1.0), [], [vm.b])
            rec.op(G_, lambda e: e.memset(vg[:, :, :, 128:129], 1.0), [], [vg.b])

            for s in range(2):
                with ExitStack() as sa:
                    w_in = mk(sa, "w_in", [128, 8, 1472], BF16, dma="sw")
                    lnE_g = mk(sa, "lnE_g", [128, D], F32, dma=True)
                    lnE_b = mk(sa, "lnE_b", [128, D], F32, dma=True)
                    for dc in range(8):
                        rec.op(G_, lambda e, dc=dc: e.dma_start(out=w_in[:, dc, :], in_=win_d[dc * 128:(dc + 1) * 128, :]),
                               [], [w_in.b] if dc == 0 else [], dsem=w_in.s)
                    w_in.b.w = (w_in.s.h, w_in.s.count, G_, True)
                    for tl, row in ((lnE_g, 0), (lnE_b, 1)):
                        dma(Q_, tl[:, :], bcast_row(vecs_d[row:row + 1, :], D), [], [tl.b], tl.s)
                    if s == 0:
                        zero_fill_init(sa)
                    B = [mk(sa, f"pB{i}", [128, 512], F32, psum=True) for i in range(6)]
                    pT0 = mk(sa, "pT0", [128, 1024], BF16, psum=True)
                    pTs = mk(sa, "pTs", [128, 1024], BF16, psum=True)
                    xt = [mk(sa, f"xt{i}", [128, D], F32, dma=True) for i in range(2)]
                    rt = [mk(sa, f"rt{i}", [128, 384], F32, dma=True) for i in range(2)]
                    xn = mk(sa, "xn", [128, D], F32)
                    h0f = [mk(sa, f"h0f{i}", [128, D], F32, dma=True) for i in range(2)]
                    h0b = mk(sa, "h0b", [128, D], BF16)
                    h0T = mk(sa, "h0T", [128, 8, 128], BF16)
                    st = mk(sa, "st", [128, 12], F32)
                    mv = mk(sa, "mv", [128, 2], F32)
                    sd = mk(sa, "sd", [128, 1], F32)
                    rstd = mk(sa, "rstd", [128, 1], F32)
                    nmr = mk(sa, "nmr", [128, 1], F32)
                    junk = mk(sa, "junk", [128, 768], F32)
                    ms2 = mk(sa, "ms2", [128, 2], F32)
                    sd2 = mk(sa, "sd2", [128, 2], F32)
                    r2 = mk(sa, "r2", [128, 2], F32)
                    qkva = mk(sa, "qkva", [128, 384], BF16)
                    qkvT = mk(sa, "qkvT", [128, 3, 128], BF16)
                    ra = mk(sa, "ra", [128, 64], F32)
                    rb = mk(sa, "rb", [128, 64], F32)
                    krd = mk(sa, "krd", [128, 128], BF16)
                    qn = mk(sa, "qn", [128, 512], BF16)
                    qa = mk(sa, "qa", [128, 4, 64], F32)
                    qb_ = mk(sa, "qb_", [128, 4, 64], F32)
                    qrf = mk(sa, "qrf", [128, 4, 64], F32)
                    qr = mk(sa, "qr", [128, 256], BF16)
                    kn = mk(sa, "kn", [128, 512], BF16)
                    gt = mk(sa, "gt", [128, 4, 128], F32)
                    ms6 = mk(sa, "ms6", [128, 6], F32)
                    sd6 = mk(sa, "sd6", [128, 6], F32)
                    r6 = mk(sa, "r6", [128, 6], F32)
                    ga = mk(sa, "ga", [128, 4, 128], F32)
                    gb = mk(sa, "gb", [128, 4, 128], F32)
                    go = mk(sa, "go", [128, 4, 128], F32)
                    qg = mk(sa, "qg", [128, 512], BF16)
                    kg = mk(sa, "kg", [128, 256], BF16)

                    junkA = mk(sa, "junkA", [128, 384], F32)

                    def load_ln(t):
                        R = 128 if t < 16 else NMETA
                        c0 = t * 128
                        xti, rti, h0fi = xt[t % 2], rt[t % 2], h0f[t % 2]
                        src_ = x_d[s, c0:c0 + 128, :] if t < 16 else meta_d[:, :]
                        dma(Q_, xti[:R, :], src_, [], [xti.b], xti.s)
                        dma(Q_, rti[:R, :], rope_d[c0:c0 + R, :], [], [rti.b], rti.s)
                        if s == 0:
                            zero_fill_chunks(4 * t, 4 * t + 4)
                        layernorm(xti, R, lnE_g, lnE_b, h0fi, xn, st, mv, sd, rstd, nmr)
                        if t < 16:
                            dma(Q_, h0s_d[c0:c0 + 128, :], h0fi[:, :], [h0fi.b], [dram_h0[t]], h0fi.s)

                    load_ln(0)
                    for t in range(_TLIM):
                        R = 128 if t < 16 else NMETA
                        c0 = t * 128
                        xti = xt[t % 2]
                        rti = rt[t % 2]
                        h0fi = h0f[t % 2]
                        rec.op(A_, lambda e, R=R, h0fi=h0fi: e.activation(out=h0b[:R, :], in_=h0fi[:R, :], func=AF.Copy),
                               [h0fi.b], [h0b.b])
                        rec.op(P_, [lambda e, dc=dc, R=R: e.transpose(out=pT0[:, dc * 128:dc * 128 + R],
                                                                       in_=h0b[:R, dc * 128:(dc + 1) * 128],
                                                                       identity=ident_b[:R, :R]) for dc in range(8)],
                               [h0b.b, ident_b.b], [pT0.b])
                        rec.op(V_, lambda e, R=R: e.tensor_copy(
                            out=h0T[:, 0:4, :R], in_=pT0[:, 0:512].rearrange("p (c r) -> p c r", c=4)[:, :, :R]),
                            [pT0.b], [h0T.b])
                        rec.op(A_, lambda e, R=R: e.activation(
                            out=h0T[:, 4:8, :R], in_=pT0[:, 512:1024].rearrange("p (c r) -> p c r", c=4)[:, :, :R],
                            func=AF.Copy), [pT0.b], [h0T.b])
                        ck("c")
                        for (a0, a1, bk) in ((0, 448, B[0]), (448, 960, B[1]), (960, 1472, B[2])):
                            rec.op(P_, [lambda e, dc=dc, R=R, a0=a0, a1=a1, bk=bk: e.matmul(
                                out=bk[:R, 0:a1 - a0], lhsT=h0T[:, dc, :R], rhs=w_in[:, dc, a0:a1],
                                start=(dc == 0), stop=(dc == 7)) for dc in range(8)],
                                [h0T.b, w_in.b], [bk.b])
                        rec._cur = rec._streams.setdefault("A", [])
                        rec.op(A_, lambda e, R=R: e.activation(out=junkA[:R, 0:256], in_=B[0][:R, 0:256], func=AF.Square,
                                                               scale=1.0 / 16.0, accum_out=ms2[:R, 0:1]),
                               [B[0].b], [junkA.b, ms2.b])
                        rec.op(A_, lambda e, R=R: e.activation(out=junkA[:R, 256:384], in_=B[0][:R, 256:384], func=AF.Square,
                                                               scale=128.0 ** -0.5, accum_out=ms2[:R, 1:2]),
                               [B[0].b], [junkA.b, ms2.b])
                        rec.op(A_, lambda e, R=R: e.activation(out=sd2[:R, :], in_=ms2[:R, :], func=AF.Sqrt,
                                                               bias=eps_rms[:R, :], scale=1.0), [ms2.b, eps_rms.b], [sd2.b])
                        rec.op(V_, lambda e, R=R: e.reciprocal(out=r2[:R, :], in_=sd2[:R, :]), [sd2.b], [r2.b])
                        rec.op(A_, lambda e, R=R: e.activation(out=qkva[:R, :], in_=B[0][:R, 0:384], func=AF.Copy),
                               [B[0].b], [qkva.b])
                        ck("f")
                        rec.op(V_, lambda e, R=R, rti=rti: e.tensor_tensor(out=ra[:R, :], in0=B[0][:R, 384:448],
                                                                             in1=rti[:R, 0:64], op=ALU.mult),
                               [B[0].b, rti.b], [ra.b])
                        rec.op(V_, lambda e, R=R, rti=rti: e.tensor_tensor(out=rb[:R, :], in0=B[0][:R, 384:448],
                                                                             in1=rti[:R, 64:128], op=ALU.mult),
                               [B[0].b, rti.b], [rb.b])
                        rec.op(V_, lambda e, R=R: e.tensor_tensor(out=krd[:R, 0:32], in0=ra[:R, 0:32], in1=rb[:R, 32:64],
                                                                  op=ALU.subtract), [ra.b, rb.b], [krd.b])
                        rec.op(V_, lambda e, R=R: e.tensor_tensor(out=krd[:R, 32:64], in0=rb[:R, 0:32], in1=ra[:R, 32:64],
                                                                  op=ALU.add), [ra.b, rb.b], [krd.b])
                        rec.op(G_, lambda e, R=R: e.tensor_copy(out=krd[:R, 64:128], in_=krd[:R, 0:64]), [krd.b], [krd.b])
                        ck("g")
                        rec.op(P_, [lambda e, c=c, R=R: e.transpose(out=pTs[:, c * 128:c * 128 + R],
                                                                     in_=qkva[:R, c * 128:(c + 1) * 128],
                                                                     identity=ident_b[:R, :R]) for c in range(3)],
                               [qkva.b, ident_b.b], [pTs.b])
                        rec.op(V_, lambda e, R=R: e.tensor_copy(
                            out=qkvT[:, :, :R], in_=pTs[:, 0:384].rearrange("p (c r) -> p c r", c=3)[:, :, :R]),
                            [pTs.b], [qkvT.b])
                        if t < 16:
                            for (bk, n0) in ((B[0], 0), (B[3], 384)):
                                rec.op(P_, [lambda e, c=c, bk=bk, n0=n0: e.matmul(
                                    out=bk[:, 0:384], lhsT=qkvT[:, c, :], rhs=w_qb[:, c, n0:n0 + 384],
                                    start=(c == 0), stop=(c == 1)) for c in range(2)], [qkvT.b, w_qb.b], [bk.b])
                        for (bk, n0) in ((B[4], 0), (B[5], 512)):
                            rec.op(P_, lambda e, bk=bk, n0=n0, R=R: e.matmul(
                                out=bk[:R, :], lhsT=qkvT[:, 2, :R], rhs=w_kvb[:, n0:n0 + 512], start=True, stop=True),
                                [qkvT.b, w_kvb.b], [bk.b])
                        ck("h")
                        if t < 16:
                            for hb, bk in ((0, B[0]), (1, B[3])):
                                for hl in range(2):
                                    rec.op(A_, lambda e, hb=hb, bk=bk, hl=hl: e.activation(
                                        out=qn[:, (hb * 2 + hl) * 128:(hb * 2 + hl + 1) * 128],
                                        in_=bk[:, hl * 192:hl * 192 + 128],
                                        func=AF.Identity, scale=r2[:, 0:1]), [bk.b, r2.b], [qn.b])
                                rec.op(V_, lambda e, hb=hb, bk=bk, rti=rti: e.tensor_tensor(
                                    out=qa[:, hb * 2:(hb + 1) * 2, :],
                                    in0=bk[:, 0:384].rearrange("p (h d) -> p h d", h=2)[:, :, 128:192],
                                    in1=rti[:, 0:64].unsqueeze(1).to_broadcast([128, 2, 64]), op=ALU.mult),
                                    [bk.b, rti.b], [qa.b])
                                rec.op(V_, lambda e, hb=hb, bk=bk, rti=rti: e.tensor_tensor(
                                    out=qb_[:, hb * 2:(hb + 1) * 2, :],
                                    in0=bk[:, 0:384].rearrange("p (h d) -> p h d", h=2)[:, :, 128:192],
                                    in1=rti[:, 64:128].unsqueeze(1).to_broadcast([128, 2, 64]), op=ALU.mult),
                                    [bk.b, rti.b], [qb_.b])
                            rec.op(V_, lambda e: e.tensor_tensor(out=qrf[:, :, 0:32], in0=qa[:, :, 0:32], in1=qb_[:, :, 32:64],
                                                                 op=ALU.subtract), [qa.b, qb_.b], [qrf.b])
                            rec.op(V_, lambda e: e.tensor_tensor(out=qrf[:, :, 32:64], in0=qb_[:, :, 0:32], in1=qa[:, :, 32:64],
                                                                 op=ALU.add), [qa.b, qb_.b], [qrf.b])
                            rec.op(V_, lambda e: e.tensor_scalar(out=qr[:, :].rearrange("p (h d) -> p h d", h=4),
                                                                 in0=qrf[:, :, :], scalar1=r2[:, 0:1], scalar2=None,
                                                                 op0=ALU.mult), [qrf.b, r2.b], [qr.b])
                            ck("i")
                            rec.op(P_, [lambda e, h=h: e.transpose(out=pTs[:, h * 128:(h + 1) * 128],
                                                                   in_=qn[:, h * 128:(h + 1) * 128], identity=ident_b[:, :])
                                        for h in range(4)] +
                                   [lambda e, h=h: e.transpose(out=pTs[:, 512 + h * 128:512 + (h + 1) * 128],
                                                               in_=qr[:, h * 128:(h + 1) * 128], identity=ident_b[:, :])
                                    for h in range(2)], [qn.b, qr.b, ident_b.b], [pTs.b])
                            rec.op(V_, lambda e, c0=c0: e.tensor_copy(
                                out=qTn[:, :, c0:c0 + 128], in_=pTs[:, 0:512].rearrange("p (h r) -> p h r", h=4)),
                                [pTs.b], [qTn.b])
                            rec.op(A_, lambda e, c0=c0: e.activation(
                                out=qTr[:, :, c0:c0 + 128], in_=pTs[:, 512:768].rearrange("p (h r) -> p h r", h=2),
                                func=AF.Copy), [pTs.b], [qTr.b])
                        ck("j")
                        for hb, bk in ((0, B[4]), (1, B[5])):
                            for hl in range(2):
                                rec.op(A_, lambda e, hb=hb, bk=bk, R=R, hl=hl: e.activation(
                                    out=kn[:R, (hb * 2 + hl) * 128:(hb * 2 + hl + 1) * 128],
                                    in_=bk[:R, hl * 256:hl * 256 + 128],
                                    func=AF.Identity, scale=r2[:R, 1:2]), [bk.b, r2.b], [kn.b])
                            rec.op(V_, lambda e, hb=hb, bk=bk, R=R, t=t: e.tensor_scalar(
                                out=vm[:R, t, hb * 2:(hb + 1) * 2, 0:128],
                                in0=bk[:R, :].rearrange("p (h d) -> p h d", h=2)[:, :, 128:256],
                                scalar1=r2[:R, 1:2], scalar2=None, op0=ALU.mult), [bk.b, r2.b], [vm.b])
                        ck("k")
                        rec.op(P_, [lambda e, h=h, R=R: e.transpose(out=pTs[:, h * 128:h * 128 + R],
                                                                     in_=kn[:R, h * 128:(h + 1) * 128],
                                                                     identity=ident_b[:R, :R]) for h in range(4)] +
                               [lambda e, R=R: e.transpose(out=pTs[:, 512:512 + R], in_=krd[:R, :], identity=ident_b[:R, :R])],
                               [kn.b, krd.b, ident_b.b], [pTs.b])
                        rec.op(V_, lambda e, R=R, c0=c0: e.tensor_copy(
                            out=kTn[:, :, c0:c0 + R], in_=pTs[:, 0:512].rearrange("p (h r) -> p h r", h=4)[:, :, :R]),
                            [pTs.b], [kTn.b])
                        rec.op(A_, lambda e, R=R, c0=c0: e.activation(out=kTr[:, c0:c0 + R], in_=pTs[:, 512:512 + R],
                                                                      func=AF.Copy), [pTs.b], [kTr.b])
                        rec._cur = rec._streams.setdefault("BC", [])
                        for j in range(2):
                            rec.op(G_, lambda e, j=j, R=R, rti=rti: e.tensor_tensor(
                                out=gt[:R, 2 * j, :], in0=rti[:R, 128:256], in1=gsm[:R, j, :], op=ALU.mult),
                                [rti.b, gsm.b], [gt.b])
                            rec.op(G_, lambda e, j=j, R=R, rti=rti: e.tensor_tensor(
                                out=gt[:R, 2 * j + 1, :], in0=rti[:R, 256:384], in1=gsm[:R, j, :], op=ALU.mult),
                                [rti.b, gsm.b], [gt.b])
                        rec.op(A_, lambda e, R=R: e.activation(out=junk[:R, 0:512], in_=B[1][:R, :], func=AF.Square,
                                                               scale=128.0 ** -0.5), [B[1].b], [junk.b])
                        rec.op(A_, lambda e, R=R: e.activation(out=junk[:R, 512:768], in_=B[2][:R, 0:256], func=AF.Square,
                                                               scale=128.0 ** -0.5), [B[2].b], [junk.b])
                        rec.op(V_, lambda e, R=R: e.tensor_reduce(out=ms6[:R, :],
                                                                  in_=junk[:R, :].rearrange("p (h d) -> p h d", h=6),
                                                                  axis=AX.X, op=ALU.add), [junk.b], [ms6.b])
                        rec.op(A_, lambda e, R=R: e.activation(out=sd6[:R, :], in_=ms6[:R, :], func=AF.Sqrt,
                                                               bias=eps_rms[:R, :], scale=1.0), [ms6.b, eps_rms.b], [sd6.b])
                        rec.op(V_, lambda e, R=R: e.reciprocal(out=r6[:R, :], in_=sd6[:R, :]), [sd6.b], [r6.b])
                        for (nh, bk, tj, dst, r0, isq) in ((4, B[1], 0, qg, 0, True), (2, B[2], 1, kg, 4, False)):
                            if isq and t == 16:
                                continue
                            rec.op(V_, lambda e, nh=nh, bk=bk, tj=tj, R=R: e.tensor_tensor(
                                out=ga[:R, 0:nh, :], in0=bk[:R, 0:nh * 128].rearrange("p (h d) -> p h d", h=nh),
                                in1=gt[:R, 2 * tj, :].unsqueeze(1).to_broadcast([R, nh, 128]), op=ALU.mult),
                                [bk.b, gt.b], [ga.b])
                            rec.op(V_, lambda e, nh=nh, bk=bk, tj=tj, R=R: e.tensor_tensor(
                                out=gb[:R, 0:nh, :], in0=bk[:R, 0:nh * 128].rearrange("p (h d) -> p h d", h=nh),
                                in1=gt[:R, 2 * tj + 1, :].unsqueeze(1).to_broadcast([R, nh, 128]), op=ALU.mult),
                                [bk.b, gt.b], [gb.b])

                            def v5(tl, nh, R):
                                return tl[:R, 0:nh, :].rearrange("p h (a b c) -> p h a b c", a=2, b=2)
                            rec.op(V_, lambda e, nh=nh, R=R: e.tensor_tensor(
                                out=v5(go, nh, R)[:, :, :, 0, :], in0=v5(ga, nh, R)[:, :, :, 0, :],
                                in1=v5(gb, nh, R)[:, :, :, 1, :], op=ALU.subtract), [ga.b, gb.b], [go.b])
                            rec.op(V_, lambda e, nh=nh, R=R: e.tensor_tensor(
                                out=v5(go, nh, R)[:, :, :, 1, :], in0=v5(gb, nh, R)[:, :, :, 0, :],
                                in1=v5(ga, nh, R)[:, :, :, 1, :], op=ALU.add), [ga.b, gb.b], [go.b])
                            rec.op(V_, lambda e, nh=nh, R=R, dst=dst, r0=r0: e.tensor_tensor(
                                out=dst[:R, 0:nh * 128].rearrange("p (h d) -> p h d", h=nh), in0=go[:R, 0:nh, :],
                                in1=r6[:R, r0:r0 + nh].unsqueeze(2).to_broadcast([R, nh, 128]), op=ALU.mult),
                                [go.b, r6.b], [dst.b])
                            rec.op(P_, [lambda e, h=h, R=R, dst=dst: e.transpose(
                                out=pT0[:, h * 128:h * 128 + R], in_=dst[:R, h * 128:(h + 1) * 128],
                                identity=ident_b[:R, :R]) for h in range(nh)], [dst.b, ident_b.b], [pT0.b])
                            if isq:
                                rec.op(V_, lambda e, c0=c0: e.tensor_copy(
                                    out=qTg[:, :, c0:c0 + 128], in_=pT0[:, 0:512].rearrange("p (h r) -> p h r", h=4)),
                                    [pT0.b], [qTg.b])
                            else:
                                rec.op(V_, lambda e, R=R, c0=c0: e.tensor_copy(
                                    out=kTg[:, :, c0:c0 + R],
                                    in_=pT0[:, 0:256].rearrange("p (h r) -> p h r", h=2)[:, :, :R]), [pT0.b], [kTg.b])
                        rec.op(A_, lambda e, R=R, t=t: e.activation(
                            out=vg[:R, t, :, 0:128], in_=B[2][:R, 256:512].rearrange("p (h d) -> p h d", h=2),
                            func=AF.Copy), [B[2].b], [vg.b])

                        rec._cur = None
                        if t + 1 < _TLIM:
                            with rec.defer("D"):
                                load_ln(t + 1)
                        rec.interleave()

                    rec.flush()
                    if _STOP == "1a":
                        raise _Stop()
                with ExitStack() as sb_:
                    w_o = mk(sb_, "w_o", [128, 8, 1024], BF16, dma="sw")
                    ln1_g = mk(sb_, "ln1_g", [128, D], F32, dma=True)
                    ln1_b = mk(sb_, "ln1_b", [128, D], F32, dma=True)
                    go_bc = mk(sb_, "go_bc", [128, D], F32, dma=True)
                    for dc in range(8):
                        rec.op(G_, lambda e, dc=dc: e.dma_start(out=w_o[:, dc, :], in_=wo_d[dc * 128:(dc + 1) * 128, :]),
                               [], [w_o.b] if dc == 0 else [], dsem=w_o.s)
                    w_o.b.w = (w_o.s.h, w_o.s.count, G_, True)
                    for tl, row in ((ln1_g, 2), (ln1_b, 3), (go_bc, 6)):
                        dma(Q_, tl[:, :], bcast_row(vecs_d[row:row + 1, :], D), [], [tl.b], tl.s)
                    S_ = [mk(sb_, f"pS{i}", [128, 512], F32, psum=True) for i in range(3)]
                    O_ = [mk(sb_, f"pO{i}", [128, 512], F32, psum=True) for i in range(4)]
                    pT7 = mk(sb_, "pT7", [128, 1024], BF16, psum=True)
                    PT = [mk(sb_, f"PT{i}", [128, 512], BF16) for i in range(3)]
                    o_sbs = [mk(sb_, f"o_sb{i}", [128, 4, D], F32) for i in range(2)]
                    rden = [mk(sb_, f"rden{i}", [128, 4], F32) for i in range(2)]
                    ostage = [mk(sb_, f"ostage{i}", [128, 4, 129], F32) for i in range(2)]
                    junkf = mk(sb_, "junkf", [128, 512], F32)
                    mso = mk(sb_, "mso", [128, 2], F32)
                    sdo = mk(sb_, "sdo", [128, 2], F32)
                    ro = mk(sb_, "ro", [128, 2], F32)
                    on = mk(sb_, "on", [128, D], BF16)
                    onT = mk(sb_, "onT", [128, 8, 128], BF16)
                    h0r = [mk(sb_, f"h0r{i}", [128, D], F32, dma=True) for i in range(2)]
                    y1 = mk(sb_, "y1", [128, D], F32)
                    xn1 = mk(sb_, "xn1", [128, D], F32)
                    h1 = [mk(sb_, f"h1_{i}", [128, D], F32, dma=True) for i in range(2)]
                    st = mk(sb_, "st1", [128, 12], F32)
                    mv = mk(sb_, "mv1", [128, 2], F32)
                    sd = mk(sb_, "sd1", [128, 1], F32)
                    rstd = mk(sb_, "rstd1", [128, 1], F32)
                    nmr = mk(sb_, "nmr1", [128, 1], F32)

                    hidx_ = [0]

                    def attention(qb):
                        q0 = qb * 512
                        o_sb = o_sbs[qb % 2]
                        hidx = hidx_[0]
                        for hh in range(8):
                            mla = hh < 4
                            h = hh % 4
                            sc = (192.0 ** -0.5) if mla else (128.0 ** -0.5)
                            vt = vm if mla else vg
                            hv = h if mla else h // 2

                            def qk(kt, mla=mla, h=h, q0=q0):
                                M = 128 if kt < 16 else NMETA
                                k0 = kt * 128
                                Sb = S_[kt % 2]
                                if mla:
                                    p0 = (h % 2) * 64
                                    fns = [lambda e: e.matmul(out=Sb[:M, :], lhsT=kTn[:, h, k0:k0 + M],
                                                              rhs=qTn[:, h, q0:q0 + 512], start=True, stop=False),
                                           lambda e: e.matmul(out=Sb[:M, :], lhsT=kTr[p0:p0 + 64, k0:k0 + M],
                                                              rhs=qTr[p0:p0 + 64, h // 2, q0:q0 + 512], start=False, stop=True)]
                                    rd = [kTn.b, kTr.b, qTn.b, qTr.b]
                                else:
                                    fns = [lambda e: e.matmul(out=Sb[:M, :], lhsT=kTg[:, h // 2, k0:k0 + M],
                                                              rhs=qTg[:, h, q0:q0 + 512], start=True, stop=True)]
                                    rd = [kTg.b, qTg.b]
                                rec.op(P_, fns, rd, [Sb.b])

                            qk(0)
                            for kt in range(17):
                                M = 128 if kt < 16 else NMETA
                                if kt + 1 < 17:
                                    qk(kt + 1)
                                Sb = S_[kt % 2]
                                Pt = PT[kt % 3]
                                rec.op(A_, lambda e, M=M, Sb=Sb, Pt=Pt, sc=sc: e.activation(
                                    out=Pt[:M, :], in_=Sb[:M, :], func=AF.Exp, scale=sc), [Sb.b], [Pt.b])
                                rec.op(P_, [lambda e, qt=qt, M=M, Pt=Pt, kt=kt, vt=vt, hv=hv: e.matmul(
                                    out=O_[qt][:, 0:129], lhsT=Pt[:M, qt * 128:(qt + 1) * 128], rhs=vt[:M, kt, hv, :],
                                    start=(kt == 0), stop=(kt == 16)) for qt in range(4)],
                                    [Pt.b, vt.b], [o.b for o in O_])
                            rd_ = rden[hidx % 2]
                            sg_ = ostage[hidx % 2]
                            hidx += 1
                            for qt in range(4):
                                if qt < 2:
                                    rec.op(V_, lambda e, qt=qt, sg_=sg_: e.tensor_copy(out=sg_[:, qt, :], in_=O_[qt][:, 0:129]),
                                           [O_[qt].b], [sg_.b])
                                else:
                                    rec.op(A_, lambda e, qt=qt, sg_=sg_: e.activation(out=sg_[:, qt, :], in_=O_[qt][:, 0:129],
                                                                                    func=AF.Copy), [O_[qt].b], [sg_.b])
                            rec.op(V_, lambda e, rd_=rd_, sg_=sg_: e.reciprocal(out=rd_[:, :].unsqueeze(2), in_=sg_[:, :, 128:129]),
                                   [sg_.b], [rd_.b])
                            rec.op(V_, lambda e, rd_=rd_, sg_=sg_, hh=hh: e.tensor_tensor(
                                out=o_sb[:, :, hh * 128:(hh + 1) * 128], in0=sg_[:, :, 0:128],
                                in1=rd_[:, :].unsqueeze(2).to_broadcast([128, 4, 128]), op=ALU.mult), [sg_.b, rd_.b], [o_sb.b])
                        hidx_[0] = hidx

                    def epilogue(qb):
                        o_sb = o_sbs[qb % 2]
                        for qt in range(4):
                            Tt = qb * 4 + qt
                            h0ri = h0r[Tt % 2]
                            h1i = h1[Tt % 2]
                            dma(Q_, h0ri[:, :], h0s_d[Tt * 128:(Tt + 1) * 128, :], [dram_h0[Tt]], [h0ri.b], h0ri.s)
                            for g in range(2):
                                rec.op(A_, lambda e, g=g, qt=qt: e.activation(
                                    out=junkf[:, :], in_=o_sb[:, qt, g * 512:(g + 1) * 512], func=AF.Square,
                                    scale=512.0 ** -0.5, accum_out=mso[:, g:g + 1]), [o_sb.b], [junkf.b, mso.b])
                            rec.op(A_, lambda e: e.activation(out=sdo[:, :], in_=mso[:, :], func=AF.Sqrt, bias=eps_rms[:, :],
                                                              scale=1.0), [mso.b, eps_rms.b], [sdo.b])
                            rec.op(V_, lambda e: e.reciprocal(out=ro[:, :], in_=sdo[:, :]), [sdo.b], [ro.b])
                            for g, en in ((0, V_), (1, V_)):
                                rec.op(en, lambda e, g=g, qt=qt: e.scalar_tensor_tensor(
                                    out=on[:, g * 512:(g + 1) * 512], in0=o_sb[:, qt, g * 512:(g + 1) * 512],
                                    scalar=ro[:, g:g + 1], in1=go_bc[:, g * 512:(g + 1) * 512], op0=ALU.mult, op1=ALU.mult),
                                    [o_sb.b, ro.b, go_bc.b], [on.b])
                            rec.op(P_, [lambda e, c=c: e.transpose(out=pT7[:, c * 128:(c + 1) * 128],
                                                                   in_=on[:, c * 128:(c + 1) * 128], identity=ident_b[:, :])
                                        for c in range(8)], [on.b, ident_b.b], [pT7.b])
                            rec.op(V_, lambda e: e.tensor_copy(out=onT[:, 0:4, :],
                                                               in_=pT7[:, 0:512].rearrange("p (c r) -> p c r", c=4)),
                                   [pT7.b], [onT.b])
                            rec.op(A_, lambda e: e.activation(out=onT[:, 4:8, :],
                                                              in_=pT7[:, 512:1024].rearrange("p (c r) -> p c r", c=4),
                                                              func=AF.Copy), [pT7.b], [onT.b])
                            for nh in range(2):
                                rec.op(P_, [lambda e, c=c, nh=nh: e.matmul(
                                    out=S_[2][:, :], lhsT=onT[:, c, :], rhs=w_o[:, c, nh * 512:(nh + 1) * 512],
                                    start=(c == 0), stop=(c == 7)) for c in range(8)], [onT.b, w_o.b], [S_[2].b])
                                rec.op(V_, lambda e, nh=nh, h0ri=h0ri: e.scalar_tensor_tensor(
                                    out=y1[:, nh * 512:(nh + 1) * 512], in0=h0ri[:, nh * 512:(nh + 1) * 512], scalar=ALPHA,
                                    in1=S_[2][:, :], op0=ALU.mult, op1=ALU.add), [h0ri.b, S_[2].b], [y1.b])
                            layernorm(y1, 128, ln1_g, ln1_b, h1i, xn1, st, mv, sd, rstd, nmr)
                            gT = s * 16 + Tt
                            dma(Q_, h1s_d[gT * 128:(gT + 1) * 128, :], h1i[:, :], [h1i.b], [dram_h1[gT]], h1i.s)

                    for qb in range(4):
                        with rec.defer("ATT"):
                            attention(qb)
                        if qb > 0:
                            with rec.defer("EPI"):
                                epilogue(qb - 1)
                        rec.interleave()
                    epilogue(3)
                    rec.flush()
                    if _STOP == "1b":
                        raise _Stop()

        w1 = [mk(es, f"w1_{i}", [128, 8, 2048], BF16, dma="sw") for i in range(2)]
        w2 = mk(es, "w2_", [128, 8, 1024], BF16, dma="sw")

        def load_w1(ex):
            wt = w1[ex % 2]
            for dc in range(8):
                rec.op(G_, lambda e, dc=dc, wt=wt, ex=ex: e.dma_start(out=wt[:, dc, :],
                                                                      in_=w1_d[ex, dc * 128:(dc + 1) * 128, :]),
                       [], [wt.b] if dc == 0 else [], dsem=wt.s)
            wt.b.w = (wt.s.h, wt.s.count, G_, True)

        def load_w2(ex):
            for fc in range(8):
                rec.op(G_, lambda e, fc=fc, ex=ex: e.dma_start(out=w2[:, fc, :], in_=w2_d[ex, fc * 128:(fc + 1) * 128, :]),
                       [], [w2.b] if fc == 0 else [], dsem=w2.s)
            w2.b.w = (w2.s.h, w2.s.count, G_, True)

        if not _SMALL:
            load_w1(0)
            load_w2(0)
            load_w1(1)
        with ExitStack() as s2:
            NB2 = 2
            RT = [[mk(s2, f"pRT{i}_{j}", [128, 512], F32, psum=True) for j in range(2)] for i in range(NB2)]
            RL = [mk(s2, f"pRL{i}", [128, 512], F32, psum=True) for i in range(NB2)]
            RP = [mk(s2, f"pRP{i}", [128, 512], F32, psum=True) for i in range(NB2)]
            wr = mk(s2, "wr", [128, 8, NE], F32, dma=True)
            br_bc = mk(s2, "br_bc", [128, NE], F32, dma=True)
            h1t = [mk(s2, f"h1t{i}", [128, D], F32, dma=True) for i in range(NB2)]
            h1b = [mk(s2, f"h1b{i}", [128, D], BF16, dma="sw") for i in range(4)]
            cum_b = mk(s2, "cum_b", [128, NE], BF16)
            ecolm = mk(s2, "ecolm", [128, NE], F32)

            def mk2(name, shape, dt):
                return [mk(s2, f"{name}{i}", shape, dt) for i in range(NB2)]
            h1T = mk2("h1T", [128, 8, 128], F32)
            lg = mk2("lg", [128, NE], F32)
            m8 = mk2("m8", [128, 8], F32)
            i8 = mk2("i8", [128, 8], U32)
            i8f = mk2("i8f", [128, 8], F32)
            mask_f = mk2("mask_f", [128, NE], F32)
            mask_b = mk2("mask_b", [128, NE], BF16)
            vld = mk2("vld", [128, NE], F32)
            sf = mk2("sf", [128, NE], F32)
            oh = [mk2(f"oh{k}_", [128, NE], F32) for k in range(4)]
            slot_f = mk2("slot_f", [128, 4], F32)
            vk = mk2("vk", [128, 4], F32)
            slot_d = [mk(s2, f"slot_d{i}", [128, 4], I32) for i in range(4)]
            slot_cf = mk2("slot_cf", [128, 4], F32)
            negm = mk2("negm", [128, 1], F32)
            e4 = mk2("e4", [128, 4], F32)
            es_ = mk2("es_", [128, 1], F32)
            res_ = mk2("res_", [128, 1], F32)
            g4 = mk2("g4", [128, 4], F32)

            dma(Q_, wr[:, :, :], wr_d.rearrange("(c p) n -> p c n", p=128), [], [wr.b], wr.s)
            dma(Q_, br_bc[:, :], bcast_row(br_d[0:1, :], NE), [], [br_bc.b], br_bc.s)
            rec.op(V_, lambda e: e.memset(cum_b[:, :], 0.0), [], [cum_b.b])
            rec.op(V_, lambda e: e.tensor_scalar(out=ecolm[:, :], in0=cst[:, 384:416], scalar1=-BIG, scalar2=None, op0=ALU.add),
                   [cst.b], [ecolm.b])

            def route_tile(Tt, i):
                ht, hb, sd_ = h1t[i], h1b[Tt % 4], slot_d[Tt % 4]
                dma(Q_, ht[:, :], h1s_d[Tt * 128:(Tt + 1) * 128, :], [dram_h1[Tt]], [ht.b], ht.s)
                rec.op(A_, lambda e: e.activation(out=hb[:, :], in_=ht[:, :], func=AF.Copy), [ht.b], [hb.b])
                yield
                for half in range(2):
                    rec.op(P_, [lambda e, c=c, half=half: e.transpose(
                        out=RT[i][half][:, c * 128:(c + 1) * 128], in_=ht[:, (half * 4 + c) * 128:(half * 4 + c + 1) * 128],
                        identity=cst[:, 0:128]) for c in range(4)], [ht.b, cst.b], [RT[i][half].b])
                yield
                rec.op(V_, lambda e: e.tensor_copy(out=h1T[i][:, 0:4, :], in_=RT[i][0][:, :].rearrange("p (c r) -> p c r", c=4)),
                       [RT[i][0].b], [h1T[i].b])
                rec.op(A_, lambda e: e.activation(out=h1T[i][:, 4:8, :], in_=RT[i][1][:, :].rearrange("p (c r) -> p c r", c=4),
                                                  func=AF.Copy), [RT[i][1].b], [h1T[i].b])
                yield
                rec.op(P_, [lambda e, c=c: e.matmul(out=RL[i][:, 0:NE], lhsT=h1T[i][:, c, :], rhs=wr[:, c, :],
                                                    start=(c == 0), stop=(c == 7)) for c in range(8)], [h1T[i].b, wr.b], [RL[i].b])
                yield
                rec.op(V_, lambda e: e.tensor_tensor(out=lg[i][:, :], in0=RL[i][:, 0:NE], in1=br_bc[:, :], op=ALU.add),
                       [RL[i].b, br_bc.b], [lg[i].b])
                yield
                rec.op(V_, lambda e: e.max(out=m8[i][:, :], in_=lg[i][:, :]), [lg[i].b], [m8[i].b])
                yield
                rec.op(V_, lambda e: e.max_index(out=i8[i][:, :], in_max=m8[i][:, :], in_values=lg[i][:, :]),
                       [m8[i].b, lg[i].b], [i8[i].b])
                rec.op(G_, lambda e: e.tensor_scalar(out=negm[i][:, :], in0=m8[i][:, 0:1], scalar1=-1.0, scalar2=None,
                                                      op0=ALU.mult), [m8[i].b], [negm[i].b])
                yield
                rec.op(V_, lambda e: e.tensor_scalar(out=mask_f[i][:, :], in0=lg[i][:, :], scalar1=m8[i][:, 3:4], scalar2=None,
                                                      op0=ALU.is_ge), [lg[i].b, m8[i].b], [mask_f[i].b])
                rec.op(A_, lambda e: e.activation(out=e4[i][:, :], in_=m8[i][:, 0:4], func=AF.Exp, bias=negm[i][:, :], scale=1.0,
                                                  accum_out=es_[i][:, :]), [m8[i].b, negm[i].b], [e4[i].b, es_[i].b])
                yield
                rec.op(G_, lambda e: e.tensor_copy(out=mask_b[i][:, :], in_=mask_f[i][:, :]), [mask_f[i].b], [mask_b[i].b])
                rec.op(V_, lambda e: e.tensor_copy(out=i8f[i][:, :], in_=i8[i][:, :]), [i8[i].b], [i8f[i].b])
                yield
                rec.op(P_, [lambda e: e.matmul(out=RP[i][:, 0:NE], lhsT=U_b[:, :], rhs=mask_b[i][:, :], start=True, stop=False),
                            lambda e: e.matmul(out=RP[i][:, 0:NE], lhsT=ones_b[:, :], rhs=cum_b[:, :], start=False, stop=True)],
                       [U_b.b, ones_b.b, mask_b[i].b, cum_b.b], [RP[i].b])
                yield
                rec.op(G_, lambda e: e.tensor_tensor(out=cum_b[:, :], in0=cum_b[:, :], in1=mask_b[i][:, :], op=ALU.add),
                       [cum_b.b, mask_b[i].b], [cum_b.b])
                rec.op(V_, lambda e: e.tensor_scalar(out=vld[i][:, :], in0=RP[i][:, 0:NE], scalar1=float(CAP), scalar2=None,
                                                      op0=ALU.is_lt), [RP[i].b], [vld[i].b])
                rec.op(V_, lambda e: e.tensor_tensor(out=sf[i][:, :], in0=RP[i][:, 0:NE], in1=ecolm[:, :], op=ALU.add),
                       [RP[i].b, ecolm.b], [sf[i].b])
                rec.op(G_, lambda e: e.reciprocal(out=res_[i][:, :], in_=es_[i][:, :]) if False else
                       e.tensor_scalar(out=res_[i][:, :], in0=es_[i][:, :], scalar1=1.0, scalar2=None, op0=ALU.mult),
                       [es_[i].b], [res_[i].b])
                yield
                rec.op(V_, lambda e: e.tensor_tensor(out=sf[i][:, :], in0=sf[i][:, :], in1=vld[i][:, :], op=ALU.mult),
                       [sf[i].b, vld[i].b], [sf[i].b])
                yield
                rec.op(V_, lambda e: e.tensor_scalar(out=sf[i][:, :], in0=sf[i][:, :], scalar1=BIG, scalar2=None, op0=ALU.add),
                       [sf[i].b], [sf[i].b])
                yield
                for k in range(4):
                    rec.op(V_, lambda e, k=k: e.scalar_tensor_tensor(out=oh[k][i][:, :], in0=cst[:, 416:448],
                                                                      scalar=i8f[i][:, k:k + 1], in1=sf[i][:, :],
                                                                      op0=ALU.is_equal, op1=ALU.mult),
                           [cst.b, i8f[i].b, sf[i].b], [oh[k][i].b])
                yield
                for k in range(4):
                    rec.op(V_, lambda e, k=k: e.tensor_reduce(out=slot_f[i][:, k:k + 1], in_=oh[k][i][:, :], axis=AX.X, op=ALU.add),
                           [oh[k][i].b], [slot_f[i].b])
                yield
                rec.op(V_, lambda e: e.tensor_scalar(out=vk[i][:, :], in0=slot_f[i][:, :], scalar1=BIG, scalar2=None, op0=ALU.is_lt),
                       [slot_f[i].b], [vk[i].b])
                rec.op(V_, lambda e: e.tensor_copy(out=sd_[:, :], in_=slot_f[i][:, :]), [slot_f[i].b], [sd_.b])
                rec.op(V_, lambda e: e.reciprocal(out=res_[i][:, :], in_=res_[i][:, :]), [res_[i].b], [res_[i].b])
                yield
                for k in range(4):
                    rec.op(G_, lambda e, k=k: e.indirect_dma_start(
                        out=xs_d[:, :], out_offset=bass.IndirectOffsetOnAxis(ap=sd_[:, k:k + 1], axis=0),
                        in_=hb[:, :], in_offset=None, bounds_check=rec.bc(e), oob_is_err=False),
                        [sd_.b, hb.b], [dram_xs] if k == 0 else [], dsem=hb.s)
                dram_xs.w = (hb.s.h, hb.s.count, G_, True)
                hb.b.r[hb.s.h] = dram_xs.w
                sd_.b.r[hb.s.h] = dram_xs.w
                rec.op(V_, lambda e: e.tensor_tensor(out=slot_cf[i][:, :], in0=slot_f[i][:, :], in1=vk[i][:, :], op=ALU.mult),
                       [slot_f[i].b, vk[i].b], [slot_cf[i].b])
                rec.op(V_, lambda e: e.tensor_scalar(out=g4[i][:, :], in0=e4[i][:, :], scalar1=res_[i][:, :], scalar2=None,
                                                      op0=ALU.mult), [e4[i].b, res_[i].b], [g4[i].b])
                yield
                rec.op(V_, lambda e: e.tensor_copy(out=slot_c_all[:, Tt, :], in_=slot_cf[i][:, :]),
                       [slot_cf[i].b], [slot_c_all.b])
                rec.op(V_, lambda e: e.tensor_tensor(out=gates_all[:, Tt, :], in0=g4[i][:, :], in1=vk[i][:, :], op=ALU.mult),
                       [g4[i].b, vk[i].b], [gates_all.b])
                yield

            pipeline(route_tile, 32, NB2, 10)
            xs_done = [(h1b[i].s.h, h1b[i].s.count, G_, True) for i in range(4)]
            rec.flush()
            if _STOP == "2a":
                raise _Stop()

        with ExitStack() as s3:
            HS = 512
            pTx = [mk(s3, f"pTx{i}", [128, 1024], BF16, psum=True) for i in range(2)]
            pG = [mk(s3, f"pG{i}", [128, 512], F32, psum=True) for i in range(2)]
            pU = [mk(s3, f"pU{i}", [128, 512], F32, psum=True) for i in range(2)]
            pY = [mk(s3, f"pY{i}", [128, 512], F32, psum=True) for i in range(2)]
            b1 = mk(s3, "b1", [128, NE, 16], F32, dma=True)
            b2bc = [mk(s3, f"b2bc{i}", [128, D], F32, dma=True) for i in range(2)]
            xsb = [mk(s3, f"xsb{i}", [128, 4, D], BF16, dma=True) for i in range(2)]
            xT = [mk(s3, f"xT{i}", [128, 8, HS], BF16) for i in range(2)]
            actT = [mk(s3, f"actT{i}", [128, 8, HS], BF16) for i in range(2)]
            g1 = [mk(s3, f"g1_{i}", [128, 512], F32) for i in range(2)]
            sg = [mk(s3, f"sg_{i}", [128, 512], F32) for i in range(2)]
            u1 = [mk(s3, f"u1_{i}", [128, 512], F32) for i in range(2)]
            tt = [mk(s3, f"tt_{i}", [128, 512], F32) for i in range(2)]
            ysb = [mk(s3, f"ysb{i}", [128, 4, D], BF16, dma=True) for i in range(2)]

            dma(Q_, b1[:, :, :], b1_d[:, :, :], [], [b1.b], b1.s)
            rec.op(V_, lambda e: e.tensor_scalar(out=b1[:, :, 8:16], in0=b1[:, :, 8:16], scalar1=1.0, scalar2=None, op0=ALU.add),
                   [b1.b], [b1.b])

            def load_x(u):
                ex, hf = divmod(u, 2)
                xb = xsb[u % 2]
                for t_ in xs_done:
                    rec.wait_tok(Q_, t_)
                r0 = ex * CAP + hf * HS
                dma(Q_, xb[:, :, :], xs_d[r0:r0 + HS, :].rearrange("(t p) d -> p t d", p=128), [dram_xs], [xb.b], xb.s)
                if hf == 0:
                    dma(Q_, b2bc[ex % 2][:, :], bcast_row(b2_d[ex:ex + 1, :], D), [], [b2bc[ex % 2].b], b2bc[ex % 2].s)

            def transpose_dc(u, dc):
                xb = xsb[u % 2]
                xTe = xT[u % 2]
                pt = pTx[dc % 2]
                rec.op(P_, [lambda e, st_=st_: e.transpose(
                    out=pt[:, st_ * 128:(st_ + 1) * 128], in_=xb[:, st_, dc * 128:(dc + 1) * 128], identity=ident_b[:, :])
                    for st_ in range(4)], [xb.b, ident_b.b], [pt.b])
                if dc % 2 == 0:
                    rec.op(V_, lambda e: e.tensor_copy(out=xTe[:, dc, :], in_=pt[:, 0:HS]), [pt.b], [xTe.b])
                else:
                    rec.op(A_, lambda e: e.activation(out=xTe[:, dc, :], in_=pt[:, 0:HS], func=AF.Copy), [pt.b], [xTe.b])

            def transposes(u):
                for dc in range(8):
                    transpose_dc(u, dc)

            def gemm1(u, tnext=None):
                ex, hf = divmod(u, 2)
                wt = w1[ex % 2]
                xTe = xT[u % 2]
                aT = actT[u % 2]
                for j in range(8):
                    pg, pu = pG[j % 2], pU[j % 2]
                    g1i, sgi, u1i, tti = g1[j % 2], sg[j % 2], u1[j % 2], tt[j % 2]
                    rec.op(P_, [lambda e, dc=dc, j=j, pg=pg: e.matmul(
                        out=pg[:, :], lhsT=wt[:, dc, j * 128:(j + 1) * 128], rhs=xTe[:, dc, :],
                        start=(dc == 0), stop=(dc == 7)) for dc in range(8)], [wt.b, xTe.b], [pg.b])
                    rec.op(P_, [lambda e, dc=dc, j=j, pu=pu: e.matmul(
                        out=pu[:, :], lhsT=wt[:, dc, 1024 + j * 128:1024 + (j + 1) * 128], rhs=xTe[:, dc, :],
                        start=(dc == 0), stop=(dc == 7)) for dc in range(8)], [wt.b, xTe.b], [pu.b])
                    rec.op(V_, lambda e, j=j, pg=pg, g1i=g1i, ex=ex: e.tensor_scalar(
                        out=g1i[:, :], in0=pg[:, :], scalar1=b1[:, ex, j:j + 1], scalar2=7.0,
                        op0=ALU.add, op1=ALU.min), [pg.b, b1.b], [g1i.b])
                    rec.op(A_, lambda e, g1i=g1i, sgi=sgi: e.activation(
                        out=sgi[:, :], in_=g1i[:, :], func=AF.Sigmoid, scale=1.702), [g1i.b], [sgi.b])
                    rec.op(V_, lambda e, j=j, pu=pu, u1i=u1i, ex=ex: e.tensor_scalar(
                        out=u1i[:, :], in0=pu[:, :], scalar1=b1[:, ex, 8 + j:9 + j], scalar2=8.0,
                        op0=ALU.add, op1=ALU.min), [pu.b, b1.b], [u1i.b])
                    rec.op(G_, lambda e, g1i=g1i, sgi=sgi, tti=tti: e.tensor_tensor(
                        out=tti[:, :], in0=g1i[:, :], in1=sgi[:, :], op=ALU.mult), [g1i.b, sgi.b], [tti.b])
                    rec.op(V_, lambda e, j=j, u1i=u1i, tti=tti, aT=aT: e.scalar_tensor_tensor(
                        out=aT[:, j, :], in0=u1i[:, :], scalar=-6.0, in1=tti[:, :],
                        op0=ALU.max, op1=ALU.mult), [u1i.b, tti.b], [aT.b])
                    if tnext is not None:
                        transpose_dc(tnext, j)

            def gemm2(u):
                ex, hf = divmod(u, 2)
                yt = ysb[u % 2]
                bb = b2bc[ex % 2]
                aT = actT[u % 2]
                i = 0
                for st_ in range(4):
                    for nh in range(2):
                        py = pY[i % 2]
                        i += 1
                        rec.op(P_, [lambda e, fc=fc, st_=st_, nh=nh, py=py: e.matmul(
                            out=py[:, :], lhsT=aT[:, fc, st_ * 128:(st_ + 1) * 128], rhs=w2[:, fc, nh * 512:(nh + 1) * 512],
                            start=(fc == 0), stop=(fc == 7)) for fc in range(8)], [aT.b, w2.b], [py.b])
                        rec.op(V_, lambda e, st_=st_, nh=nh, py=py, yt=yt, bb=bb: e.tensor_tensor(
                            out=yt[:, st_, nh * 512:(nh + 1) * 512], in0=py[:, :], in1=bb[:, nh * 512:(nh + 1) * 512],
                            op=ALU.add), [py.b, bb.b], [yt.b])
                r0 = ex * CAP + hf * HS
                dma(Q_, ys_d[r0:r0 + HS, :].rearrange("(t p) d -> p t d", p=128), yt[:, :, :], [yt.b], [], yt.s)

            NU = 2 * NE
            load_x(0)
            transposes(0)
            for u in range(NU):
                ex, hf = divmod(u, 2)
                if hf == 0 and 1 <= ex and ex + 1 < NE:
                    load_w1(ex + 1)
                if u + 1 < NU:
                    load_x(u + 1)
                gemm1(u, tnext=(u + 1 if u + 1 < NU else None))
                gemm2(u)
                if hf == 1 and ex + 1 < NE:
                    load_w2(ex + 1)
            ys_done = [(ysb[i].s.h, ysb[i].s.count, Q_, True) for i in range(2)]
            rec.flush()
            if _STOP == "2b":
                raise _Stop()

        with ExitStack() as s4:
            NB3 = 4
            ln2_g = mk(s4, "ln2_g", [128, D], F32, dma=True)
            ln2_b = mk(s4, "ln2_b", [128, D], F32, dma=True)
            h1c = [mk(s4, f"h1c{i}", [128, D], F32, dma=True) for i in range(NB3)]
            yg = [mk(s4, f"yg{i}", [128, 4, D], BF16, dma="sw") for i in range(NB3)]
            acc = [mk(s4, f"acc{i}", [128, D], F32) for i in range(NB3)]
            xn2 = [mk(s4, f"xn2{i}", [128, D], F32) for i in range(NB3)]
            ot = [mk(s4, f"ot{i}", [128, D], F32, dma=True) for i in range(NB3)]
            st = [mk(s4, f"st2{i}", [128, 12], F32) for i in range(NB3)]
            mv = [mk(s4, f"mv2{i}", [128, 2], F32) for i in range(NB3)]
            sd = [mk(s4, f"sd2_{i}", [128, 1], F32) for i in range(NB3)]
            rstd = [mk(s4, f"rstd2{i}", [128, 1], F32) for i in range(NB3)]
            nmr = [mk(s4, f"nmr2{i}", [128, 1], F32) for i in range(NB3)]
            dma(Q_, ln2_g[:, :], bcast_row(vecs_d[4:5, :], D), [], [ln2_g.b], ln2_g.s)
            dma(Q_, ln2_b[:, :], bcast_row(vecs_d[5:6, :], D), [], [ln2_b.b], ln2_b.s)
            for t_ in ys_done:
                rec.wait_tok(G_, t_)
            out_toks = []

            def comb_tile(Tt, i):
                hc, ygi, oti, acci = h1c[i], yg[i], ot[i], acc[i]
                dma(Q_, hc[:, :], h1s_d[Tt * 128:(Tt + 1) * 128, :], [], [hc.b], hc.s)
                for k in range(4):
                    rec.op(G_, lambda e, k=k: e.indirect_dma_start(
                        out=ygi[:, k, :], out_offset=None, in_=ys_d[:, :],
                        in_offset=bass.IndirectOffsetOnAxis(ap=slot_c_all[:, Tt, k:k + 1], axis=0),
                        bounds_check=rec.bc(e), oob_is_err=False),
                        [slot_c_all.b], [ygi.b] if k == 0 else [], dsem=ygi.s)
                ygi.b.w = (ygi.s.h, ygi.s.count, G_, True)
                yield
                rec.op(A_, lambda e: e.activation(out=acci[:, :], in_=hc[:, :], func=AF.Copy, scale=ALPHA), [hc.b], [acci.b])
                yield
                for k in range(4):
                    rec.op(V_, lambda e, k=k: e.scalar_tensor_tensor(
                        out=acci[:, :], in0=ygi[:, k, :], scalar=gates_all[:, Tt, k:k + 1], in1=acci[:, :],
                        op0=ALU.mult, op1=ALU.add), [ygi.b, gates_all.b, acci.b], [acci.b])
                    yield
                yield from layernorm_g(acci, 128, ln2_g, ln2_b, oti, xn2[i], st[i], mv[i], sd[i], rstd[i], nmr[i], gmul=G_)
                sq_, tq_ = divmod(Tt, 16)
                out_toks.append(dma(Q_, out_d[sq_, tq_ * 128:(tq_ + 1) * 128, :], oti[:, :], [oti.b], [], oti.s))
                yield

            pipeline(comb_tile, 32, NB3, 4)
            for tk in out_toks[-4:]:
                rec.wait_tok(Q_, tk)
            for e_ in (P_, A_, V_, G_):
                if rec.cnt[e_] > 0:
                    i = rec.cnt[e_] - 1
                    rec.wait_tok(Q_, (rec.esem[e_][i // CH], i % CH + 1, e_, False))
            rec.flush()
            if _STOP == "3":
                raise _Stop()

    except _Stop:
        pass
    return nc


def _rope_tables():
    theta = np.float32(10000.0)

    def cs(pos, dim):
        inv = (theta ** (-(np.arange(0, dim, 2, dtype=np.float32) / np.float32(dim)))).astype(np.float32)
        ang = (pos.astype(np.float32)[:, None] * inv[None, :]).astype(np.float32)
        return np.cos(ang.astype(np.float64)).astype(np.float32), np.sin(ang.astype(np.float64)).astype(np.float32)

    pos1 = np.concatenate([np.arange(NMETA, L), np.arange(NMETA)])
    row = np.concatenate([np.arange(SEQ) // 64, np.full(NMETA, -1)])
    col = np.concatenate([np.arange(SEQ) % 64, np.arange(NMETA)])
    c1, s1 = cs(pos1, 64)
    cr, sr = cs(row, 64)
    cc, sc = cs(col, 64)
    return np.concatenate([c1, c1, s1, s1, cr, cr, cc, cc, sr, sr, sc, sc], axis=1).astype(np.float32)


_NC_CACHE = {}


def kernel(x, meta_tokens, ln_emb_g, ln_emb_b, w_in, g_q_a, w_q_b, g_kv_a, w_kv_b,
           g_q_gqa, g_k_gqa, g_o_mla, g_o_gqa, w_o, ln1_g, ln1_b,
           w_router, b_router, w_gate_up, b_gate_up, w_down, b_down, ln2_g, ln2_b):
    f = lambda a: np.ascontiguousarray(np.asarray(a, dtype=np.float32))
    x = f(x)
    vecs = np.zeros((8, D), np.float32)
    vecs[0], vecs[1] = f(ln_emb_g), f(ln_emb_b)
    vecs[2], vecs[3] = f(ln1_g)[0], f(ln1_b)[0]
    vecs[4], vecs[5] = f(ln2_g)[0], f(ln2_b)[0]
    vecs[6, :512], vecs[6, 512:] = f(g_o_mla)[0], f(g_o_gqa)[0]
    gsm = np.stack([f(g_q_gqa)[0], f(g_k_gqa)[0]])
    gpp = np.zeros((128, 4), np.float32)
    gpp[:, 0], gpp[:, 1] = f(g_q_a)[0, :128], f(g_q_a)[0, 128:]
    gpp[:, 2] = f(g_kv_a)[0]
    wgu = f(w_gate_up)[0]
    w1 = np.ascontiguousarray(np.concatenate([wgu[:, :, 0::2], wgu[:, :, 1::2]], axis=2))
    bgu = f(b_gate_up)[0]
    b1 = np.concatenate([bgu[:, 0::2], bgu[:, 1::2]], axis=1)
    b1t = np.ascontiguousarray(b1.reshape(NE, 16, 128).transpose(2, 0, 1))
    cst = np.zeros((128, 448), np.float32)
    cst[:, 0:128] = np.eye(128, dtype=np.float32)
    cst[:, 128:256] = np.triu(np.ones((128, 128), np.float32), 1)
    cst[:, 256:384] = 1.0
    cst[:, 384:416] = (np.arange(NE, dtype=np.float32) * CAP)[None, :]
    cst[:, 416:448] = np.arange(NE, dtype=np.float32)[None, :]
    shared = {
        "meta": f(meta_tokens), "vecs": vecs, "gsm": gsm, "gpp": gpp,
        "w_in": f(w_in)[0], "w_qb": f(w_q_b)[0], "w_kvb": f(w_kv_b)[0], "w_o": f(w_o)[0],
        "w_r": f(w_router)[0], "b_r": f(b_router), "w1": w1 if not _SMALL else w1[:1], "b1t": b1t,
        "w2": f(w_down)[0] if not _SMALL else f(w_down)[0][:1],
        "b2": f(b_down)[0], "rope": _rope_tables(), "cst": cst,
    }
    if "nc" not in _NC_CACHE:
        _NC_CACHE["nc"] = build_nc()
    nc = _NC_CACHE["nc"]
    in_maps = []
    for c in range(NCORES):
        m = dict(shared)
        m["x"] = np.ascontiguousarray(x[2 * c:2 * c + 2])
        in_maps.append(m)
    res = run_bass_kernel_spmd(nc, in_maps, core_ids=list(range(NCORES)))
    return np.concatenate([np.asarray(r["out"], dtype=np.float32) for r in res.results], axis=0)
```

```python
import numpy as np
from contextlib import ExitStack
import concourse.bass as bass
import concourse.mybir as mybir
from concourse.bass_utils import run_bass_kernel_spmd

F32 = mybir.dt.float32
BF16 = mybir.dt.bfloat16
I32 = mybir.dt.int32
U32 = mybir.dt.uint32
AF = mybir.ActivationFunctionType
ALU = mybir.AluOpType
AX = mybir.AxisListType

NCORES = 8
SEQ = 2048
NMETA = 16
L = SEQ + NMETA
D = 1024
NE = 32
CAP = 1024
NSLOT = NE * CAP
BIG = float(1 << 20)
ALPHA = 2.0 ** 0.25
RMS_EPS = 1e-6
LN_EPS = 1e-5
CH = 4000

P_, A_, V_, G_, Q_ = "pe", "act", "dve", "pool", "sp"


import os
_STOP = os.environ.get("K_STOP", "")
_SMALL = _STOP in ("s0", "1a", "1b", "2a")
_TLIM = int(os.environ.get("K_TLIM", "17"))
_SUB = os.environ.get("K_SUB", "")


class _Stop(Exception):
    pass


class Buf:
    def __init__(self, name):
        self.name = name
        self.w = None
        self.r = {}


class DSem:
    def __init__(self, h):
        self.h = h
        self.count = 0


class Rec:
    def __init__(self, nc, stack):
        self.nc = nc
        self.stack = stack
        self.prog = {e: [] for e in (P_, A_, V_, G_, Q_)}
        self.cnt = {e: 0 for e in self.prog}
        self.esem = {e: [] for e in self.prog}
        self.seen = {e: {} for e in self.prog}
        self.nsem = 0
        self.dsems = []
        self.free_dsems = {}
        self._cur = None
        self._streams = {}

    def new_sem(self, name):
        self.nsem += 1
        return self.stack.enter_context(self.nc.semaphore(name))

    def dsem(self, name, stack=None, kind="hw"):
        pool = self.free_dsems.setdefault(kind, [])
        if pool:
            d = pool.pop()
        else:
            d = DSem(self.new_sem(name))
            self.dsems.append(d)
        if stack is not None:
            stack.callback(lambda d=d, pool=pool: pool.append(d))
        return d

    def flush(self):
        self.barrier()
        with self.nc.Block() as blk:
            @blk.sync
            def _(e):
                for f in self.prog[Q_]:
                    f(e)

            @blk.tensor
            def _(e):
                for f in self.prog[P_]:
                    f(e)

            @blk.scalar
            def _(e):
                for f in self.prog[A_]:
                    f(e)

            @blk.vector
            def _(e):
                for f in self.prog[V_]:
                    f(e)

            @blk.gpsimd
            def _(e):
                self.bc_reg = None
                for f in self.prog[G_]:
                    f(e)
        for e in self.prog:
            self.prog[e] = []

    def bc(self, e):
        if self.bc_reg is None:
            self.bc_reg = e.to_reg(NSLOT - 1)
        return self.bc_reg

    def barrier(self):
        toks = []
        for e2 in self.prog:
            if self.cnt[e2] > 0:
                i = self.cnt[e2] - 1
                toks.append((self.esem[e2][i // CH], i % CH + 1, e2, False))
        for d in self.dsems:
            if d.count > 0:
                toks.append((d.h, d.count, None, True))
        for e in self.prog:
            for t in toks:
                if t[2] == e and not t[3]:
                    continue
                self._wait(e, t)

    def _etoken(self, e):
        i = self.cnt[e]
        k = i // CH
        while len(self.esem[e]) <= k:
            self.esem[e].append(self.new_sem(f"e_{e}{len(self.esem[e])}"))
        self.cnt[e] += 1
        return (self.esem[e][k], i % CH + 1, e, False)

    def _wait(self, e, tok):
        sem, val = tok[0], tok[1]
        if self.seen[e].get(sem, 0) >= val:
            return
        self.seen[e][sem] = val
        self.prog[e].append(lambda eng, sem=sem, val=val: eng.wait_ge(sem, val))

    def defer(self, name):
        rec = self

        class _D:
            def __enter__(s_):
                rec._cur = rec._streams.setdefault(name, [])

            def __exit__(s_, *a):
                rec._cur = None
                return False
        return _D()

    def interleave(self):
        lists = [l for l in self._streams.values() if l]
        self._streams = {}
        idx = [0] * len(lists)
        alive = True
        while alive:
            alive = False
            for i, l in enumerate(lists):
                if idx[i] < len(l):
                    self.op(*l[idx[i]])
                    idx[i] += 1
                    alive = True

    def op(self, e, fns, reads=(), writes=(), dsem=None):
        if self._cur is not None:
            self._cur.append((e, fns, list(reads), list(writes), dsem))
            return None
        if callable(fns):
            fns = [fns]
        for b in reads:
            if b.w is not None:
                self._wait(e, b.w)
            if getattr(b, "psum", False):
                for t in b.r.values():
                    if t[2] != e:
                        self._wait(e, t)
        for b in writes:
            if b.w is not None and (b.w[2] != e or b.w[3] or e != P_):
                self._wait(e, b.w)
            for t in b.r.values():
                if t[2] != e or t[3] or e != P_:
                    self._wait(e, t)
        if dsem is None:
            tok = self._etoken(e)
            inc = 1
        else:
            dsem.count += 16
            tok = (dsem.h, dsem.count, e, True)
            inc = 16
        n = len(fns)
        for i, fn in enumerate(fns):
            if i == n - 1:
                self.prog[e].append(lambda eng, fn=fn, s=tok[0], inc=inc: fn(eng).then_inc(s, inc))
            else:
                self.prog[e].append(lambda eng, fn=fn: fn(eng))
        for b in reads:
            b.r[tok[0]] = tok
        for b in writes:
            b.w = tok
            b.r = {}
        return tok

    def wait_tok(self, e, tok):
        self._wait(e, tok)


class T:
    _n = [0]

    def __init__(self, rec, stack, nc, name, shape, dt, psum=False, dma=False):
        T._n[0] += 1
        name = f"t{T._n[0]}_{name}"
        if psum:
            self.t = stack.enter_context(nc.psum_tensor(name, list(shape), dt))
        else:
            self.t = stack.enter_context(nc.sbuf_tensor(name, list(shape), dt))
        self.b = Buf(name)
        self.b.psum = psum
        self.s = rec.dsem("d_" + name, stack, "sw" if dma == "sw" else "hw") if dma else None

    def __getitem__(self, k):
        return self.t[k]


def build_nc():
    nc = bass.Bass("TRN2", target_bir_lowering=False)

    def din(name, shape, dt=F32):
        return nc.dram_tensor(name, list(shape), dt, kind="ExternalInput").ap()

    x_d = din("x", [2, SEQ, D])
    meta_d = din("meta", [NMETA, D])
    vecs_d = din("vecs", [8, D])
    gsm_d = din("gsm", [2, 128])
    gpp_d = din("gpp", [128, 4])
    win_d = din("w_in", [D, 1472])
    wqb_d = din("w_qb", [256, 768])
    wkvb_d = din("w_kvb", [128, 1024])
    wo_d = din("w_o", [D, D])
    wr_d = din("w_r", [D, NE])
    br_d = din("b_r", [1, NE])
    w1_d = din("w1", [NE if not _SMALL else 1, D, 2048])
    b1_d = din("b1t", [128, NE, 16])
    w2_d = din("w2", [NE if not _SMALL else 1, D, D])
    b2_d = din("b2", [NE, D])
    rope_d = din("rope", [L, 384])
    cst_d = din("cst", [128, 448])
    out_d = nc.dram_tensor("out", [2, SEQ, D], F32, kind="ExternalOutput").ap()
    h0s_d = nc.dram_tensor("h0s", [SEQ, D], F32).ap()
    h1s_d = nc.dram_tensor("h1s", [2 * SEQ, D], F32).ap()
    xs_d = nc.dram_tensor("xs", [NSLOT, D], BF16).ap()
    ys_d = nc.dram_tensor("ys", [NSLOT, D], BF16).ap()

    es = ExitStack()
    try:
      with es:
        rec = Rec(nc, es)

        def mk(stack, name, shape, dt, psum=False, dma=False):
            return T(rec, stack, nc, name, shape, dt, psum=psum, dma=dma)

        def dma(e, out, in_, reads, writes, sem):
            return rec.op(e, lambda eng: eng.dma_start(out=out, in_=in_), reads=reads, writes=writes, dsem=sem)

        def ck(tag):
            if _SUB == tag:
                rec.flush()
                raise _Stop()

        cst = mk(es, "cst", [128, 448], F32, dma=True)
        ident_b = mk(es, "ident_b", [128, 128], BF16)
        U_b = mk(es, "U_b", [128, 128], BF16)
        ones_b = mk(es, "ones_b", [128, 128], BF16)
        gpp = mk(es, "gpp", [128, 4], F32, dma=True)
        gsm = mk(es, "gsm", [128, 2, 128], F32, dma=True)
        eps_ln = mk(es, "eps_ln", [128, 1], F32)
        eps_rms = mk(es, "eps_rms", [128, 1], F32)
        slot_c_all = mk(es, "slot_c_all", [128, 32, 4], I32)
        gates_all = mk(es, "gates_all", [128, 32, 4], F32)
        dram_xs = Buf("xs")
        dram_ys = Buf("ys")
        dram_h0 = [Buf(f"h0s{i}") for i in range(16)]
        dram_h1 = [Buf(f"h1s{i}") for i in range(32)]

        dma(Q_, cst[:, :], cst_d[:, :], [], [cst.b], cst.s)
        dma(Q_, gpp[:, :], gpp_d[:, :], [], [gpp.b], gpp.s)
        dma(Q_, gsm[:, :, :], gsm_d.unsqueeze(0).to_broadcast([128, 2, 128]), [], [gsm.b], gsm.s)
        rec.op(V_, lambda e: e.tensor_copy(out=ident_b[:, :], in_=cst[:, 0:128]), [cst.b], [ident_b.b])
        rec.op(V_, lambda e: e.tensor_copy(out=U_b[:, :], in_=cst[:, 128:256]), [cst.b], [U_b.b])
        rec.op(V_, lambda e: e.tensor_copy(out=ones_b[:, :], in_=cst[:, 256:384]), [cst.b], [ones_b.b])
        rec.op(V_, lambda e: e.memset(eps_ln[:, :], LN_EPS), [], [eps_ln.b])
        rec.op(V_, lambda e: e.memset(eps_rms[:, :], RMS_EPS), [], [eps_rms.b])
        xs_v = xs_d.rearrange("(k p a) d -> k p a d", p=128, a=4)

        zf = {}

        def zero_fill_init(stack):
            zf["b"] = mk(stack, "zero_b", [128, 4096], BF16, dma=True)
            rec.op(G_, lambda e: e.memset(zf["b"][:, :], 0.0), [], [zf["b"].b])

        def zero_fill_chunks(k0, k1):
            zb = zf["b"]
            for k in range(k0, min(k1, NSLOT // 512)):
                dma(Q_, xs_v[k], zb[:, :].rearrange("p (a d) -> p a d", a=4), [zb.b], [dram_xs], zb.s)

        def bcast_row(ap_row, n):
            return ap_row.to_broadcast([128, n])

        def layernorm_g(src, R, g_bc, b_bc, dst, tmp, st, mv, sd, rstd, nmr, gmul=V_):
            rec.op(V_, lambda e: e.bn_stats(out=st[:R, 0:6], in_=src[:R, 0:512]), [src.b], [st.b])
            yield
            rec.op(V_, lambda e: e.bn_stats(out=st[:R, 6:12], in_=src[:R, 512:1024]), [src.b], [st.b])
            yield
            rec.op(V_, lambda e: e.bn_aggr(out=mv[:R, :], in_=st[:R, :]), [st.b], [mv.b])
            yield
            rec.op(A_, lambda e: e.activation(out=sd[:R, :], in_=mv[:R, 1:2], func=AF.Sqrt, bias=eps_ln[:R, :], scale=1.0),
                   [mv.b, eps_ln.b], [sd.b])
            yield
            rec.op(V_, lambda e: e.reciprocal(out=rstd[:R, :], in_=sd[:R, :]), [sd.b], [rstd.b])
            yield
            rec.op(V_, lambda e: e.tensor_scalar(out=nmr[:R, :], in0=mv[:R, 0:1], scalar1=rstd[:R, :], scalar2=-1.0,
                                                  op0=ALU.mult, op1=ALU.mult), [mv.b, rstd.b], [nmr.b])
            yield
            rec.op(A_, lambda e: e.activation(out=tmp[:R, :], in_=src[:R, :], func=AF.Identity, bias=nmr[:R, :], scale=rstd[:R, :]),
                   [src.b, nmr.b, rstd.b], [tmp.b])
            yield
            rec.op(gmul, lambda e: e.tensor_tensor(out=tmp[:R, :], in0=tmp[:R, :], in1=g_bc[:R, :], op=ALU.mult),
                   [tmp.b, g_bc.b], [tmp.b])
            yield
            rec.op(G_, lambda e: e.tensor_tensor(out=dst[:R, :], in0=tmp[:R, :], in1=b_bc[:R, :], op=ALU.add),
                   [tmp.b, b_bc.b], [dst.b])
            yield

        def layernorm(*a, **k):
            for _ in layernorm_g(*a, **k):
                pass

        def pipeline(make_gen, n, nb, stag):
            active = {}
            free = list(range(nb))
            nxt = 0
            rnd = 0
            while nxt < n or active:
                if nxt < n and free and rnd % stag == 0:
                    i = free.pop(0)
                    active[i] = make_gen(nxt, i)
                    nxt += 1
                for i in list(active):
                    try:
                        next(active[i])
                    except StopIteration:
                        del active[i]
                        free.append(i)
                rnd += 1

        def lockstep(gens):
            gens = list(gens)
            while gens:
                for g_ in list(gens):
                    try:
                        next(g_)
                    except StopIteration:
                        gens.remove(g_)

        with ExitStack() as s1:
            w_qb = mk(s1, "w_qb", [128, 2, 768], BF16)
            w_kvb = mk(s1, "w_kvb", [128, 1024], BF16)
            with ExitStack() as s0:
                stg = mk(s0, "stg_qb", [128, 2, 768], F32, dma=True)
                stg2 = mk(s0, "stg_kvb", [128, 1024], F32, dma=True)
                dma(Q_, stg[:, :, :], wqb_d.rearrange("(c p) n -> p c n", p=128), [], [stg.b], stg.s)
                dma(Q_, stg2[:, :], wkvb_d[:, :], [], [stg2.b], stg2.s)
                for c in range(2):
                    rec.op(V_, lambda e, c=c: e.tensor_scalar(out=w_qb[:, c, :], in0=stg[:, c, :], scalar1=gpp[:, c:c + 1],
                                                               scalar2=None, op0=ALU.mult), [stg.b, gpp.b], [w_qb.b])
                rec.op(V_, lambda e: e.tensor_scalar(out=w_kvb[:, :], in0=stg2[:, :], scalar1=gpp[:, 2:3], scalar2=None,
                                                      op0=ALU.mult), [stg2.b, gpp.b], [w_kvb.b])
                rec.flush()
                if _STOP == "s0":
                    raise _Stop()

            qTn = mk(s1, "qTn", [128, 4, SEQ], BF16)
            qTr = mk(s1, "qTr", [128, 2, SEQ], BF16)
            qTg = mk(s1, "qTg", [128, 4, SEQ], BF16)
            kTn = mk(s1, "kTn", [128, 4, L], BF16)
            kTr = mk(s1, "kTr", [128, L], BF16)
            kTg = mk(s1, "kTg", [128, 2, L], BF16)
            vm = mk(s1, "vm", [128, 17, 4, 129], BF16)
            vg = mk(s1, "vg", [128, 17, 2, 129], BF16)
            rec.op(G_, lambda e: e.memset(vm[:, :, :, 128:129], 1.0), [], [vm.b])
            rec.op(G_, lambda e: e.memset(vg[:, :, :, 128:129], 1.0), [], [vg.b])

            for s in range(2):
                with ExitStack() as sa:
                    w_in = mk(sa, "w_in", [128, 8, 1472], BF16, dma="sw")
                    lnE_g = mk(sa, "lnE_g", [128, D], F32, dma=True)
                    lnE_b = mk(sa, "lnE_b", [128, D], F32, dma=True)
                    for dc in range(8):
                        rec.op(G_, lambda e, dc=dc: e.dma_start(out=w_in[:, dc, :], in_=win_d[dc * 128:(dc + 1) * 128, :]),
                               [], [w_in.b] if dc == 0 else [], dsem=w_in.s)
                    w_in.b.w = (w_in.s.h, w_in.s.count, G_, True)
                    for tl, row in ((lnE_g, 0), (lnE_b, 1)):
                        dma(Q_, tl[:, :], bcast_row(vecs_d[row:row + 1, :], D), [], [tl.b], tl.s)
                    if s == 0:
                        zero_fill_init(sa)
                    B = [mk(sa, f"pB{i}", [128, 512], F32, psum=True) for i in range(6)]
                    pT0 = mk(sa, "pT0", [128, 1024], BF16, psum=True)
                    pTs = mk(sa, "pTs", [128, 1024], BF16, psum=True)
                    xt = [mk(sa, f"xt{i}", [128, D], F32, dma=True) for i in range(2)]
                    rt = [mk(sa, f"rt{i}", [128, 384], F32, dma=True) for i in range(2)]
                    xn = mk(sa, "xn", [128, D], F32)
                    h0f = [mk(sa, f"h0f{i}", [128, D], F32, dma=True) for i in range(2)]
                    h0b = mk(sa, "h0b", [128, D], BF16)
                    h0T = mk(sa, "h0T", [128, 8, 128], BF16)
                    st = mk(sa, "st", [128, 12], F32)
                    mv = mk(sa, "mv", [128, 2], F32)
                    sd = mk(sa, "sd", [128, 1], F32)
                    rstd = mk(sa, "rstd", [128, 1], F32)
                    nmr = mk(sa, "nmr", [128, 1], F32)
                    junk = mk(sa, "junk", [128, 768], F32)
                    ms2 = mk(sa, "ms2", [128, 2], F32)
                    sd2 = mk(sa, "sd2", [128, 2], F32)
                    r2 = mk(sa, "r2", [128, 2], F32)
                    qkva = mk(sa, "qkva", [128, 384], BF16)
                    qkvT = mk(sa, "qkvT", [128, 3, 128], BF16)
                    ra = mk(sa, "ra", [128, 64], F32)
                    rb = mk(sa, "rb", [128, 64], F32)
                    krd = mk(sa, "krd", [128, 128], BF16)
                    qn = mk(sa, "qn", [128, 512], BF16)
                    qa = mk(sa, "qa", [128, 4, 64], F32)
                    qb_ = mk(sa, "qb_", [128, 4, 64], F32)
                    qrf = mk(sa, "qrf", [128, 4, 64], F32)
                    qr = mk(sa, "qr", [128, 256], BF16)
                    kn = mk(sa, "kn", [128, 512], BF16)
                    gt = mk(sa, "gt", [128, 4, 128], F32)
                    ms6 = mk(sa, "ms6", [128, 6], F32)
                    sd6 = mk(sa, "sd6", [128, 6], F32)
                    r6 = mk(sa, "r6", [128, 6], F32)
                    ga = mk(sa, "ga", [128, 4, 128], F32)
                    gb = mk(sa, "gb", [128, 4, 128], F32)
                    go = mk(sa, "go", [128, 4, 128], F32)
                    qg = mk(sa, "qg", [128, 512], BF16)
                    kg = mk(sa, "kg", [128, 256], BF16)

                    junkA = mk(sa, "junkA", [128, 384], F32)

                    def load_ln(t):
                        R = 128 if t < 16 else NMETA
                        c0 = t * 128
                        xti, rti, h0fi = xt[t % 2], rt[t % 2], h0f[t % 2]
                        src_ = x_d[s, c0:c0 + 128, :] if t < 16 else meta_d[:, :]
                        dma(Q_, xti[:R, :], src_, [], [xti.b], xti.s)
                        dma(Q_, rti[:R, :], rope_d[c0:c0 + R, :], [], [rti.b], rti.s)
                        if s == 0:
                            zero_fill_chunks(4 * t, 4 * t + 4)
                        layernorm(xti, R, lnE_g, lnE_b, h0fi, xn, st, mv, sd, rstd, nmr)
                        if t < 16:
                            dma(Q_, h0s_d[c0:c0 + 128, :], h0fi[:, :], [h0fi.b], [dram_h0[t]], h0fi.s)

                    load_ln(0)
                    for t in range(_TLIM):
                        R = 128 if t < 16 else NMETA
                        c0 = t * 128
                        xti = xt[t % 2]
                        rti = rt[t % 2]
                        h0fi = h0f[t % 2]
                        rec.op(A_, lambda e, R=R, h0fi=h0fi: e.activation(out=h0b[:R, :], in_=h0fi[:R, :], func=AF.Copy),
                               [h0fi.b], [h0b.b])
                        rec.op(P_, [lambda e, dc=dc, R=R: e.transpose(out=pT0[:, dc * 128:dc * 128 + R],
                                                                       in_=h0b[:R, dc * 128:(dc + 1) * 128],
                                                                       identity=ident_b[:R, :R]) for dc in range(8)],
                               [h0b.b, ident_b.b], [pT0.b])
                        rec.op(V_, lambda e, R=R: e.tensor_copy(
                            out=h0T[:, 0:4, :R], in_=pT0[:, 0:512].rearrange("p (c r) -> p c r", c=4)[:, :, :R]),
                            [pT0.b], [h0T.b])
                        rec.op(A_, lambda e, R=R: e.activation(
                            out=h0T[:, 4:8, :R], in_=pT0[:, 512:1024].rearrange("p (c r) -> p c r", c=4)[:, :, :R],
                            func=AF.Copy), [pT0.b], [h0T.b])
                        ck("c")
                        for (a0, a1, bk) in ((0, 448, B[0]), (448, 960, B[1]), (960, 1472, B[2])):
                            rec.op(P_, [lambda e, dc=dc, R=R, a0=a0, a1=a1, bk=bk: e.matmul(
                                out=bk[:R, 0:a1 - a0], lhsT=h0T[:, dc, :R], rhs=w_in[:, dc, a0:a1],
                                start=(dc == 0), stop=(dc == 7)) for dc in range(8)],
                                [h0T.b, w_in.b], [bk.b])
                        rec._cur = rec._streams.setdefault("A", [])
                        rec.op(A_, lambda e, R=R: e.activation(out=junkA[:R, 0:256], in_=B[0][:R, 0:256], func=AF.Square,
                                                               scale=1.0 / 16.0, accum_out=ms2[:R, 0:1]),
                               [B[0].b], [junkA.b, ms2.b])
                        rec.op(A_, lambda e, R=R: e.activation(out=junkA[:R, 256:384], in_=B[0][:R, 256:384], func=AF.Square,
                                                               scale=128.0 ** -0.5, accum_out=ms2[:R, 1:2]),
                               [B[0].b], [junkA.b, ms2.b])
                        rec.op(A_, lambda e, R=R: e.activation(out=sd2[:R, :], in_=ms2[:R, :], func=AF.Sqrt,
                                                               bias=eps_rms[:R, :], scale=1.0), [ms2.b, eps_rms.b], [sd2.b])
                        rec.op(V_, lambda e, R=R: e.reciprocal(out=r2[:R, :], in_=sd2[:R, :]), [sd2.b], [r2.b])
                        rec.op(A_, lambda e, R=R: e.activation(out=qkva[:R, :], in_=B[0][:R, 0:384], func=AF.Copy),
                               [B[0].b], [qkva.b])
                        ck("f")
                        rec.op(V_, lambda e, R=R, rti=rti: e.tensor_tensor(out=ra[:R, :], in0=B[0][:R, 384:448],
                                                                             in1=rti[:R, 0:64], op=ALU.mult),
                               [B[0].b, rti.b], [ra.b])
                        rec.op(V_, lambda e, R=R, rti=rti: e.tensor_tensor(out=rb[:R, :], in0=B[0][:R, 384:448],
                                                                             in1=rti[:R, 64:128], op=ALU.mult),
                               [B[0].b, rti.b], [rb.b])
                        rec.op(V_, lambda e, R=R: e.tensor_tensor(out=krd[:R, 0:32], in0=ra[:R, 0:32], in1=rb[:R, 32:64],
                                                                  op=ALU.subtract), [ra.b, rb.b], [krd.b])
                        rec.op(V_, lambda e, R=R: e.tensor_tensor(out=krd[:R, 32:64], in0=rb[:R, 0:32], in1=ra[:R, 32:64],
                                                                  op=ALU.add), [ra.b, rb.b], [krd.b])
                        rec.op(G_, lambda e, R=R: e.tensor_copy(out=krd[:R, 64:128], in_=krd[:R, 0:64]), [krd.b], [krd.b])
                        ck("g")
                        rec.op(P_, [lambda e, c=c, R=R: e.transpose(out=pTs[:, c * 128:c * 128 + R],
                                                                     in_=qkva[:R, c * 128:(c + 1) * 128],
                                                                     identity=ident_b[:R, :R]) for c in range(3)],
                               [qkva.b, ident_b.b], [pTs.b])
                        rec.op(V_, lambda e, R=R: e.tensor_copy(
                            out=qkvT[:, :, :R], in_=pTs[:, 0:384].rearrange("p (c r) -> p c r", c=3)[:, :, :R]),
                            [pTs.b], [qkvT.b])
                        if t < 16:
                            for (bk, n0) in ((B[0], 0), (B[3], 384)):
                                rec.op(P_, [lambda e, c=c, bk=bk, n0=n0: e.matmul(
                                    out=bk[:, 0:384], lhsT=qkvT[:, c, :], rhs=w_qb[:, c, n0:n0 + 384],
                                    start=(c == 0), stop=(c == 1)) for c in range(2)], [qkvT.b, w_qb.b], [bk.b])
                        for (bk, n0) in ((B[4], 0), (B[5], 512)):
                            rec.op(P_, lambda e, bk=bk, n0=n0, R=R: e.matmul(
                                out=bk[:R, :], lhsT=qkvT[:, 2, :R], rhs=w_kvb[:, n0:n0 + 512], start=True, stop=True),
                                [qkvT.b, w_kvb.b], [bk.b])
                        ck("h")
                        if t < 16:
                            for hb, bk in ((0, B[0]), (1, B[3])):
                                for hl in range(2):
                                    rec.op(A_, lambda e, hb=hb, bk=bk, hl=hl: e.activation(
                                        out=qn[:, (hb * 2 + hl) * 128:(hb * 2 + hl + 1) * 128],
                                        in_=bk[:, hl * 192:hl * 192 + 128],
                                        func=AF.Identity, scale=r2[:, 0:1]), [bk.b, r2.b], [qn.b])
                                rec.op(V_, lambda e, hb=hb, bk=bk, rti=rti: e.tensor_tensor(
                                    out=qa[:, hb * 2:(hb + 1) * 2, :],
                                    in0=bk[:, 0:384].rearrange("p (h d) -> p h d", h=2)[:, :, 128:192],
                                    in1=rti[:, 0:64].unsqueeze(1).to_broadcast([128, 2, 64]), op=ALU.mult),
                                    [bk.b, rti.b], [qa.b])
                                rec.op(V_, lambda e, hb=hb, bk=bk, rti=rti: e.tensor_tensor(
                                    out=qb_[:, hb * 2:(hb + 1) * 2, :],
                                    in0=bk[:, 0:384].rearrange("p (h d) -> p h d", h=2)[:, :, 128:192],
                                    in1=rti[:, 64:128].unsqueeze(1).to_broadcast([128, 2, 64]), op=ALU.mult),
                                    [bk.b, rti.b], [qb_.b])
                            rec.op(V_, lambda e: e.tensor_tensor(out=qrf[:, :, 0:32], in0=qa[:, :, 0:32], in1=qb_[:, :, 32:64],
                                                                 op=ALU.subtract), [qa.b, qb_.b], [qrf.b])
                            rec.op(V_, lambda e: e.tensor_tensor(out=qrf[:, :, 32:64], in0=qb_[:, :, 0:32], in1=qa[:, :, 32:64],
                                                                 op=ALU.add), [qa.b, qb_.b], [qrf.b])
                            rec.op(V_, lambda e: e.tensor_scalar(out=qr[:, :].rearrange("p (h d) -> p h d", h=4),
                                                                 in0=qrf[:, :, :], scalar1=r2[:, 0:1], scalar2=None,
                                                                 op0=ALU.mult), [qrf.b, r2.b], [qr.b])
                            ck("i")
                            rec.op(P_, [lambda e, h=h: e.transpose(out=pTs[:, h * 128:(h + 1) * 128],
                                                                   in_=qn[:, h * 128:(h + 1) * 128], identity=ident_b[:, :])
                                        for h in range(4)] +
                                   [lambda e, h=h: e.transpose(out=pTs[:, 512 + h * 128:512 + (h + 1) * 128],
                                                               in_=qr[:, h * 128:(h + 1) * 128], identity=ident_b[:, :])
                                    for h in range(2)], [qn.b, qr.b, ident_b.b], [pTs.b])
                            rec.op(V_, lambda e, c0=c0: e.tensor_copy(
                                out=qTn[:, :, c0:c0 + 128], in_=pTs[:, 0:512].rearrange("p (h r) -> p h r", h=4)),
                                [pTs.b], [qTn.b])
                            rec.op(A_, lambda e, c0=c0: e.activation(
                                out=qTr[:, :, c0:c0 + 128], in_=pTs[:, 512:768].rearrange("p (h r) -> p h r", h=2),
                                func=AF.Copy), [pTs.b], [qTr.b])
                        ck("j")
                        for hb, bk in ((0, B[4]), (1, B[5])):
                            for hl in range(2):
                                rec.op(A_, lambda e, hb=hb, bk=bk, R=R, hl=hl: e.activation(
                                    out=kn[:R, (hb * 2 + hl) * 128:(hb * 2 + hl + 1) * 128],
                                    in_=bk[:R, hl * 256:hl * 256 + 128],
                                    func=AF.Identity, scale=r2[:R, 1:2]), [bk.b, r2.b], [kn.b])
                            rec.op(V_, lambda e, hb=hb, bk=bk, R=R, t=t: e.tensor_scalar(
                                out=vm[:R, t, hb * 2:(hb + 1) * 2, 0:128],
                                in0=bk[:R, :].rearrange("p (h d) -> p h d", h=2)[:, :, 128:256],
                                scalar1=r2[:R, 1:2], scalar2=None, op0=ALU.mult), [bk.b, r2.b], [vm.b])
                        ck("k")
                        rec.op(P_, [lambda e, h=h, R=R: e.transpose(out=pTs[:, h * 128:h * 128 + R],
                                                                     in_=kn[:R, h * 128:(h + 1) * 128],
                                                                     identity=ident_b[:R, :R]) for h in range(4)] +
                               [lambda e, R=R: e.transpose(out=pTs[:, 512:512 + R], in_=krd[:R, :], identity=ident_b[:R, :R])],
                               [kn.b, krd.b, ident_b.b], [pTs.b])
                        rec.op(V_, lambda e, R=R, c0=c0: e.tensor_copy(
                            out=kTn[:, :, c0:c0 + R], in_=pTs[:, 0:512].rearrange("p (h r) -> p h r", h=4)[:, :, :R]),
                            [pTs.b], [kTn.b])
                        rec.op(A_, lambda e, R=R, c0=c0: e.activation(out=kTr[:, c0:c0 + R], in_=pTs[:, 512:512 + R],
                                                                      func=AF.Copy), [pTs.b], [kTr.b])
                        rec._cur = rec._streams.setdefault("BC", [])
                        for j in range(2):
                            rec.op(G_, lambda e, j=j, R=R, rti=rti: e.tensor_tensor(
                                out=gt[:R, 2 * j, :], in0=rti[:R, 128:256], in1=gsm[:R, j, :], op=ALU.mult),
                                [rti.b, gsm.b], [gt.b])
                            rec.op(G_, lambda e, j=j, R=R, rti=rti: e.tensor_tensor(
                                out=gt[:R, 2 * j + 1, :], in0=rti[:R, 256:384], in1=gsm[:R, j, :], op=ALU.mult),
                                [rti.b, gsm.b], [gt.b])
                        rec.op(A_, lambda e, R=R: e.activation(out=junk[:R, 0:512], in_=B[1][:R, :], func=AF.Square,
                                                               scale=128.0 ** -0.5), [B[1].b], [junk.b])
                        rec.op(A_, lambda e, R=R: e.activation(out=junk[:R, 512:768], in_=B[2][:R, 0:256], func=AF.Square,
                                                               scale=128.0 ** -0.5), [B[2].b], [junk.b])
                        rec.op(V_, lambda e, R=R: e.tensor_reduce(out=ms6[:R, :],
                                                                  in_=junk[:R, :].rearrange("p (h d) -> p h d", h=6),
                                                                  axis=AX.X, op=ALU.add), [junk.b], [ms6.b])
                        rec.op(A_, lambda e, R=R: e.activation(out=sd6[:R, :], in_=ms6[:R, :], func=AF.Sqrt,
                                                               bias=eps_rms[:R, :], scale=1.0), [ms6.b, eps_rms.b], [sd6.b])
                        rec.op(V_, lambda e, R=R: e.reciprocal(out=r6[:R, :], in_=sd6[:R, :]), [sd6.b], [r6.b])
                        for (nh, bk, tj, dst, r0, isq) in ((4, B[1], 0, qg, 0, True), (2, B[2], 1, kg, 4, False)):
                            if isq and t == 16:
                                continue
                            rec.op(V_, lambda e, nh=nh, bk=bk, tj=tj, R=R: e.tensor_tensor(
                                out=ga[:R, 0:nh, :], in0=bk[:R, 0:nh * 128].rearrange("p (h d) -> p h d", h=nh),
                                in1=gt[:R, 2 * tj, :].unsqueeze(1).to_broadcast([R, nh, 128]), op=ALU.mult),
                                [bk.b, gt.b], [ga.b])
                            rec.op(V_, lambda e, nh=nh, bk=bk, tj=tj, R=R: e.tensor_tensor(
                                out=gb[:R, 0:nh, :], in0=bk[:R, 0:nh * 128].rearrange("p (h d) -> p h d", h=nh),
                                in1=gt[:R, 2 * tj + 1, :].unsqueeze(1).to_broadcast([R, nh, 128]), op=ALU.mult),
                                [bk.b, gt.b], [gb.b])

                            def v5(tl, nh, R):
                                return tl[:R, 0:nh, :].rearrange("p h (a b c) -> p h a b c", a=2, b=2)
                            rec.op(V_, lambda e, nh=nh, R=R: e.tensor_tensor(
                                out=v5(go, nh, R)[:, :, :, 0, :], in0=v5(ga, nh, R)[:, :, :, 0, :],
                                in1=v5(gb, nh, R)[:, :, :, 1, :], op=ALU.subtract), [ga.b, gb.b], [go.b])
                            rec.op(V_, lambda e, nh=nh, R=R: e.tensor_tensor(
                                out=v5(go, nh, R)[:, :, :, 1, :], in0=v5(gb, nh, R)[:, :, :, 0, :],
                                in1=v5(ga, nh, R)[:, :, :, 1, :], op=ALU.add), [ga.b, gb.b], [go.b])
                            rec.op(V_, lambda e, nh=nh, R=R, dst=dst, r0=r0: e.tensor_tensor(
                                out=dst[:R, 0:nh * 128].rearrange("p (h d) -> p h d", h=nh), in0=go[:R, 0:nh, :],
                                in1=r6[:R, r0:r0 + nh].unsqueeze(2).to_broadcast([R, nh, 128]), op=ALU.mult),
                                [go.b, r6.b], [dst.b])
                            rec.op(P_, [lambda e, h=h, R=R, dst=dst: e.transpose(
                                out=pT0[:, h * 128:h * 128 + R], in_=dst[:R, h * 128:(h + 1) * 128],
                                identity=ident_b[:R, :R]) for h in range(nh)], [dst.b, ident_b.b], [pT0.b])
                            if isq:
                                rec.op(V_, lambda e, c0=c0: e.tensor_copy(
                                    out=qTg[:, :, c0:c0 + 128], in_=pT0[:, 0:512].rearrange("p (h r) -> p h r", h=4)),
                                    [pT0.b], [qTg.b])
                            else:
                                rec.op(V_, lambda e, R=R, c0=c0: e.tensor_copy(
                                    out=kTg[:, :, c0:c0 + R],
                                    in_=pT0[:, 0:256].rearrange("p (h r) -> p h r", h=2)[:, :, :R]), [pT0.b], [kTg.b])
                        rec.op(A_, lambda e, R=R, t=t: e.activation(
                            out=vg[:R, t, :, 0:128], in_=B[2][:R, 256:512].rearrange("p (h d) -> p h d", h=2),
                            func=AF.Copy), [B[2].b], [vg.b])

                        rec._cur = None
                        if t + 1 < _TLIM:
                            with rec.defer("D"):
                                load_ln(t + 1)
                        rec.interleave()

                    rec.flush()
                    if _STOP == "1a":
                        raise _Stop()
                with ExitStack() as sb_:
                    w_o = mk(sb_, "w_o", [128, 8, 1024], BF16, dma="sw")
                    ln1_g = mk(sb_, "ln1_g", [128, D], F32, dma=True)
                    ln1_b = mk(sb_, "ln1_b", [128, D], F32, dma=True)
                    go_bc = mk(sb_, "go_bc", [128, D], F32, dma=True)
                    for dc in range(8):
                        rec.op(G_, lambda e, dc=dc: e.dma_start(out=w_o[:, dc, :], in_=wo_d[dc * 128:(dc + 1) * 128, :]),
                               [], [w_o.b] if dc == 0 else [], dsem=w_o.s)
                    w_o.b.w = (w_o.s.h, w_o.s.count, G_, True)
                    for tl, row in ((ln1_g, 2), (ln1_b, 3), (go_bc, 6)):
                        dma(Q_, tl[:, :], bcast_row(vecs_d[row:row + 1, :], D), [], [tl.b], tl.s)
                    S_ = [mk(sb_, f"pS{i}", [128, 512], F32, psum=True) for i in range(3)]
                    O_ = [mk(sb_, f"pO{i}", [128, 512], F32, psum=True) for i in range(4)]
                    pT7 = mk(sb_, "pT7", [128, 1024], BF16, psum=True)
                    PT = [mk(sb_, f"PT{i}", [128, 512], BF16) for i in range(3)]
                    o_sbs = [mk(sb_, f"o_sb{i}", [128, 4, D], F32) for i in range(2)]
                    rden = [mk(sb_, f"rden{i}", [128, 4], F32) for i in range(2)]
                    ostage = [mk(sb_, f"ostage{i}", [128, 4, 129], F32) for i in range(2)]
                    junkf = mk(sb_, "junkf", [128, 512], F32)
                    mso = mk(sb_, "mso", [128, 2], F32)
                    sdo = mk(sb_, "sdo", [128, 2], F32)
                    ro = mk(sb_, "ro", [128, 2], F32)
                    on = mk(sb_, "on", [128, D], BF16)
                    onT = mk(sb_, "onT", [128, 8, 128], BF16)
                    h0r = [mk(sb_, f"h0r{i}", [128, D], F32, dma=True) for i in range(2)]
                    y1 = mk(sb_, "y1", [128, D], F32)
                    xn1 = mk(sb_, "xn1", [128, D], F32)
                    h1 = [mk(sb_, f"h1_{i}", [128, D], F32, dma=True) for i in range(2)]
                    st = mk(sb_, "st1", [128, 12], F32)
                    mv = mk(sb_, "mv1", [128, 2], F32)
                    sd = mk(sb_, "sd1", [128, 1], F32)
                    rstd = mk(sb_, "rstd1", [128, 1], F32)
                    nmr = mk(sb_, "nmr1", [128, 1], F32)

                    hidx_ = [0]

                    def attention(qb):
                        q0 = qb * 512
                        o_sb = o_sbs[qb % 2]
                        hidx = hidx_[0]
                        for hh in range(8):
                            mla = hh < 4
                            h = hh % 4
                            sc = (192.0 ** -0.5) if mla else (128.0 ** -0.5)
                            vt = vm if mla else vg
                            hv = h if mla else h // 2

                            def qk(kt, mla=mla, h=h, q0=q0):
                                M = 128 if kt < 16 else NMETA
                                k0 = kt * 128
                                Sb = S_[kt % 2]
                                if mla:
                                    p0 = (h % 2) * 64
                                    fns = [lambda e: e.matmul(out=Sb[:M, :], lhsT=kTn[:, h, k0:k0 + M],
                                                              rhs=qTn[:, h, q0:q0 + 512], start=True, stop=False),
                                           lambda e: e.matmul(out=Sb[:M, :], lhsT=kTr[p0:p0 + 64, k0:k0 + M],
                                                              rhs=qTr[p0:p0 + 64, h // 2, q0:q0 + 512], start=False, stop=True)]
                                    rd = [kTn.b, kTr.b, qTn.b, qTr.b]
                                else:
                                    fns = [lambda e: e.matmul(out=Sb[:M, :], lhsT=kTg[:, h // 2, k0:k0 + M],
                                                              rhs=qTg[:, h, q0:q0 + 512], start=True, stop=True)]
                                    rd = [kTg.b, qTg.b]
                                rec.op(P_, fns, rd, [Sb.b])

                            qk(0)
                            for kt in range(17):
                                M = 128 if kt < 16 else NMETA
                                if kt + 1 < 17:
                                    qk(kt + 1)
                                Sb = S_[kt % 2]
                                Pt = PT[kt % 3]
                                rec.op(A_, lambda e, M=M, Sb=Sb, Pt=Pt, sc=sc: e.activation(
                                    out=Pt[:M, :], in_=Sb[:M, :], func=AF.Exp, scale=sc), [Sb.b], [Pt.b])
                                rec.op(P_, [lambda e, qt=qt, M=M, Pt=Pt, kt=kt, vt=vt, hv=hv: e.matmul(
                                    out=O_[qt][:, 0:129], lhsT=Pt[:M, qt * 128:(qt + 1) * 128], rhs=vt[:M, kt, hv, :],
                                    start=(kt == 0), stop=(kt == 16)) for qt in range(4)],
                                    [Pt.b, vt.b], [o.b for o in O_])
                            rd_ = rden[hidx % 2]
                            sg_ = ostage[hidx % 2]
                            hidx += 1
                            for qt in range(4):
                                if qt < 2:
                                    rec.op(V_, lambda e, qt=qt, sg_=sg_: e.tensor_copy(out=sg_[:, qt, :], in_=O_[qt][:, 0:129]),
                                           [O_[qt].b], [sg_.b])
                                else:
                                    rec.op(A_, lambda e, qt=qt, sg_=sg_: e.activation(out=sg_[:, qt, :], in_=O_[qt][:, 0:129],
                                                                                    func=AF.Copy), [O_[qt].b], [sg_.b])
                            rec.op(V_, lambda e, rd_=rd_, sg_=sg_: e.reciprocal(out=rd_[:, :].unsqueeze(2), in_=sg_[:, :, 128:129]),
                                   [sg_.b], [rd_.b])
                            rec.op(V_, lambda e, rd_=rd_, sg_=sg_, hh=hh: e.tensor_tensor(
                                out=o_sb[:, :, hh * 128:(hh + 1) * 128], in0=sg_[:, :, 0:128],
                                in1=rd_[:, :].unsqueeze(2).to_broadcast([128, 4, 128]), op=ALU.mult), [sg_.b, rd_.b], [o_sb.b])
                        hidx_[0] = hidx

                    def epilogue(qb):
                        o_sb = o_sbs[qb % 2]
                        for qt in range(4):
                            Tt = qb * 4 + qt
                            h0ri = h0r[Tt % 2]
                            h1i = h1[Tt % 2]
                            dma(Q_, h0ri[:, :], h0s_d[Tt * 128:(Tt + 1) * 128, :], [dram_h0[Tt]], [h0ri.b], h0ri.s)
                            for g in range(2):
                                rec.op(A_, lambda e, g=g, qt=qt: e.activation(
                                    out=junkf[:, :], in_=o_sb[:, qt, g * 512:(g + 1) * 512], func=AF.Square,
                                    scale=512.0 ** -0.5, accum_out=mso[:, g:g + 1]), [o_sb.b], [junkf.b, mso.b])
                            rec.op(A_, lambda e: e.activation(out=sdo[:, :], in_=mso[:, :], func=AF.Sqrt, bias=eps_rms[:, :],
                                                              scale=1.0), [mso.b, eps_rms.b], [sdo.b])
                            rec.op(V_, lambda e: e.reciprocal(out=ro[:, :], in_=sdo[:, :]), [sdo.b], [ro.b])
                            for g, en in ((0, V_), (1, V_)):
                                rec.op(en, lambda e, g=g, qt=qt: e.scalar_tensor_tensor(
                                    out=on[:, g * 512:(g + 1) * 512], in0=o_sb[:, qt, g * 512:(g + 1) * 512],
                                    scalar=ro[:, g:g + 1], in1=go_bc[:, g * 512:(g + 1) * 512], op0=ALU.mult, op1=ALU.mult),
                                    [o_sb.b, ro.b, go_bc.b], [on.b])
                            rec.op(P_, [lambda e, c=c: e.transpose(out=pT7[:, c * 128:(c + 1) * 128],
                                                                   in_=on[:, c * 128:(c + 1) * 128], identity=ident_b[:, :])
                                        for c in range(8)], [on.b, ident_b.b], [pT7.b])
                            rec.op(V_, lambda e: e.tensor_copy(out=onT[:, 0:4, :],
                                                               in_=pT7[:, 0:512].rearrange("p (c r) -> p c r", c=4)),
                                   [pT7.b], [onT.b])
                            rec.op(A_, lambda e: e.activation(out=onT[:, 4:8, :],
                                                              in_=pT7[:, 512:1024].rearrange("p (c r) -> p c r", c=4),
                                                              func=AF.Copy), [pT7.b], [onT.b])
                            for nh in range(2):
                                rec.op(P_, [lambda e, c=c, nh=nh: e.matmul(
                                    out=S_[2][:, :], lhsT=onT[:, c, :], rhs=w_o[:, c, nh * 512:(nh + 1) * 512],
                                    start=(c == 0), stop=(c == 7)) for c in range(8)], [onT.b, w_o.b], [S_[2].b])
                                rec.op(V_, lambda e, nh=nh, h0ri=h0ri: e.scalar_tensor_tensor(
                                    out=y1[:, nh * 512:(nh + 1) * 512], in0=h0ri[:, nh * 512:(nh + 1) * 512], scalar=ALPHA,
                                    in1=S_[2][:, :], op0=ALU.mult, op1=ALU.add), [h0ri.b, S_[2].b], [y1.b])
                            layernorm(y1, 128, ln1_g, ln1_b, h1i, xn1, st, mv, sd, rstd, nmr)
                            gT = s * 16 + Tt
                            dma(Q_, h1s_d[gT * 128:(gT + 1) * 128, :], h1i[:, :], [h1i.b], [dram_h1[gT]], h1i.s)

                    for qb in range(4):
                        with rec.defer("ATT"):
                            attention(qb)
                        if qb > 0:
                            with rec.defer("EPI"):
                                epilogue(qb - 1)
                        rec.interleave()
                    epilogue(3)
                    rec.flush()
                    if _STOP == "1b":
                        raise _Stop()

        w1 = [mk(es, f"w1_{i}", [128, 8, 2048], BF16, dma="sw") for i in range(2)]
        w2 = mk(es, "w2_", [128, 8, 1024], BF16, dma="sw")

        def load_w1(ex):
            wt = w1[ex % 2]
            for dc in range(8):
                rec.op(G_, lambda e, dc=dc, wt=wt, ex=ex: e.dma_start(out=wt[:, dc, :],
                                                                      in_=w1_d[ex, dc * 128:(dc + 1) * 128, :]),
                       [], [wt.b] if dc == 0 else [], dsem=wt.s)
            wt.b.w = (wt.s.h, wt.s.count, G_, True)

        def load_w2(ex):
            for fc in range(8):
                rec.op(G_, lambda e, fc=fc, ex=ex: e.dma_start(out=w2[:, fc, :], in_=w2_d[ex, fc * 128:(fc + 1) * 128, :]),
                       [], [w2.b] if fc == 0 else [], dsem=w2.s)
            w2.b.w = (w2.s.h, w2.s.count, G_, True)

        if not _SMALL:
            load_w1(0)
            load_w2(0)
            load_w1(1)
        with ExitStack() as s2:
            NB2 = 2
            RT = [[mk(s2, f"pRT{i}_{j}", [128, 512], F32, psum=True) for j in range(2)] for i in range(NB2)]
            RL = [mk(s2, f"pRL{i}", [128, 512], F32, psum=True) for i in range(NB2)]
            RP = [mk(s2, f"pRP{i}", [128, 512], F32, psum=True) for i in range(NB2)]
            wr = mk(s2, "wr", [128, 8, NE], F32, dma=True)
            br_bc = mk(s2, "br_bc", [128, NE], F32, dma=True)
            h1t = [mk(s2, f"h1t{i}", [128, D], F32, dma=True) for i in range(NB2)]
            h1b = [mk(s2, f"h1b{i}", [128, D], BF16, dma="sw") for i in range(4)]
            cum_b = mk(s2, "cum_b", [128, NE], BF16)
            ecolm = mk(s2, "ecolm", [128, NE], F32)

            def mk2(name, shape, dt):
                return [mk(s2, f"{name}{i}", shape, dt) for i in range(NB2)]
            h1T = mk2("h1T", [128, 8, 128], F32)
            lg = mk2("lg", [128, NE], F32)
            m8 = mk2("m8", [128, 8], F32)
            i8 = mk2("i8", [128, 8], U32)
            i8f = mk2("i8f", [128, 8], F32)
            mask_f = mk2("mask_f", [128, NE], F32)
            mask_b = mk2("mask_b", [128, NE], BF16)
            vld = mk2("vld", [128, NE], F32)
            sf = mk2("sf", [128, NE], F32)
            oh = [mk2(f"oh{k}_", [128, NE], F32) for k in range(4)]
            slot_f = mk2("slot_f", [128, 4], F32)
            vk = mk2("vk", [128, 4], F32)
            slot_d = [mk(s2, f"slot_d{i}", [128, 4], I32) for i in range(4)]
            slot_cf = mk2("slot_cf", [128, 4], F32)
            negm = mk2("negm", [128, 1], F32)
            e4 = mk2("e4", [128, 4], F32)
            es_ = mk2("es_", [128, 1], F32)
            res_ = mk2("res_", [128, 1], F32)
            g4 = mk2("g4", [128, 4], F32)

            dma(Q_, wr[:, :, :], wr_d.rearrange("(c p) n -> p c n", p=128), [], [wr.b], wr.s)
            dma(Q_, br_bc[:, :], bcast_row(br_d[0:1, :], NE), [], [br_bc.b], br_bc.s)
            rec.op(V_, lambda e: e.memset(cum_b[:, :], 0.0), [], [cum_b.b])
            rec.op(V_, lambda e: e.tensor_scalar(out=ecolm[:, :], in0=cst[:, 384:416], scalar1=-BIG, scalar2=None, op0=ALU.add),
                   [cst.b], [ecolm.b])

            def route_tile(Tt, i):
                ht, hb, sd_ = h1t[i], h1b[Tt % 4], slot_d[Tt % 4]
                dma(Q_, ht[:, :], h1s_d[Tt * 128:(Tt + 1) * 128, :], [dram_h1[Tt]], [ht.b], ht.s)
                rec.op(A_, lambda e: e.activation(out=hb[:, :], in_=ht[:, :], func=AF.Copy), [ht.b], [hb.b])
                yield
                for half in range(2):
                    rec.op(P_, [lambda e, c=c, half=half: e.transpose(
                        out=RT[i][half][:, c * 128:(c + 1) * 128], in_=ht[:, (half * 4 + c) * 128:(half * 4 + c + 1) * 128],
                        identity=cst[:, 0:128]) for c in range(4)], [ht.b, cst.b], [RT[i][half].b])
                yield
                rec.op(V_, lambda e: e.tensor_copy(out=h1T[i][:, 0:4, :], in_=RT[i][0][:, :].rearrange("p (c r) -> p c r", c=4)),
                       [RT[i][0].b], [h1T[i].b])
                rec.op(A_, lambda e: e.activation(out=h1T[i][:, 4:8, :], in_=RT[i][1][:, :].rearrange("p (c r) -> p c r", c=4),
                                                  func=AF.Copy), [RT[i][1].b], [h1T[i].b])
                yield
                rec.op(P_, [lambda e, c=c: e.matmul(out=RL[i][:, 0:NE], lhsT=h1T[i][:, c, :], rhs=wr[:, c, :],
                                                    start=(c == 0), stop=(c == 7)) for c in range(8)], [h1T[i].b, wr.b], [RL[i].b])
                yield
                rec.op(V_, lambda e: e.tensor_tensor(out=lg[i][:, :], in0=RL[i][:, 0:NE], in1=br_bc[:, :], op=ALU.add),
                       [RL[i].b, br_bc.b], [lg[i].b])
                yield
                rec.op(V_, lambda e: e.max(out=m8[i][:, :], in_=lg[i][:, :]), [lg[i].b], [m8[i].b])
                yield
                rec.op(V_, lambda e: e.max_index(out=i8[i][:, :], in_max=m8[i][:, :], in_values=lg[i][:, :]),
                       [m8[i].b, lg[i].b], [i8[i].b])
                rec.op(G_, lambda e: e.tensor_scalar(out=negm[i][:, :], in0=m8[i][:, 0:1], scalar1=-1.0, scalar2=None,
                                                      op0=ALU.mult), [m8[i].b], [negm[i].b])
                yield
                rec.op(V_, lambda e: e.tensor_scalar(out=mask_f[i][:, :], in0=lg[i][:, :], scalar1=m8[i][:, 3:4], scalar2=None,
                                                      op0=ALU.is_ge), [lg[i].b, m8[i].b], [mask_f[i].b])
                rec.op(A_, lambda e: e.activation(out=e4[i][:, :], in_=m8[i][:, 0:4], func=AF.Exp, bias=negm[i][:, :], scale=1.0,
                                                  accum_out=es_[i][:, :]), [m8[i].b, negm[i].b], [e4[i].b, es_[i].b])
                yield
                rec.op(G_, lambda e: e.tensor_copy(out=mask_b[i][:, :], in_=mask_f[i][:, :]), [mask_f[i].b], [mask_b[i].b])
                rec.op(V_, lambda e: e.tensor_copy(out=i8f[i][:, :], in_=i8[i][:, :]), [i8[i].b], [i8f[i].b])
                yield
                rec.op(P_, [lambda e: e.matmul(out=RP[i][:, 0:NE], lhsT=U_b[:, :], rhs=mask_b[i][:, :], start=True, stop=False),
                            lambda e: e.matmul(out=RP[i][:, 0:NE], lhsT=ones_b[:, :], rhs=cum_b[:, :], start=False, stop=True)],
                       [U_b.b, ones_b.b, mask_b[i].b, cum_b.b], [RP[i].b])
                yield
                rec.op(G_, lambda e: e.tensor_tensor(out=cum_b[:, :], in0=cum_b[:, :], in1=mask_b[i][:, :], op=ALU.add),
                       [cum_b.b, mask_b[i].b], [cum_b.b])
                rec.op(V_, lambda e: e.tensor_scalar(out=vld[i][:, :], in0=RP[i][:, 0:NE], scalar1=float(CAP), scalar2=None,
                                                      op0=ALU.is_lt), [RP[i].b], [vld[i].b])
                rec.op(V_, lambda e: e.tensor_tensor(out=sf[i][:, :], in0=RP[i][:, 0:NE], in1=ecolm[:, :], op=ALU.add),
                       [RP[i].b, ecolm.b], [sf[i].b])
                rec.op(G_, lambda e: e.reciprocal(out=res_[i][:, :], in_=es_[i][:, :]) if False else
                       e.tensor_scalar(out=res_[i][:, :], in0=es_[i][:, :], scalar1=1.0, scalar2=None, op0=ALU.mult),
                       [es_[i].b], [res_[i].b])
                yield
                rec.op(V_, lambda e: e.tensor_tensor(out=sf[i][:, :], in0=sf[i][:, :], in1=vld[i][:, :], op=ALU.mult),
                       [sf[i].b, vld[i].b], [sf[i].b])
                yield
                rec.op(V_, lambda e: e.tensor_scalar(out=sf[i][:, :], in0=sf[i][:, :], scalar1=BIG, scalar2=None, op0=ALU.add),
                       [sf[i].b], [sf[i].b])
                yield
                for k in range(4):
                    rec.op(V_, lambda e, k=k: e.scalar_tensor_tensor(out=oh[k][i][:, :], in0=cst[:, 416:448],
                                                                      scalar=i8f[i][:, k:k + 1], in1=sf[i][:, :],
                                                                      op0=ALU.is_equal, op1=ALU.mult),
                           [cst.b, i8f[i].b, sf[i].b], [oh[k][i].b])
                yield
                for k in range(4):
                    rec.op(V_, lambda e, k=k: e.tensor_reduce(out=slot_f[i][:, k:k + 1], in_=oh[k][i][:, :], axis=AX.X, op=ALU.add),
                           [oh[k][i].b], [slot_f[i].b])
                yield
                rec.op(V_, lambda e: e.tensor_scalar(out=vk[i][:, :], in0=slot_f[i][:, :], scalar1=BIG, scalar2=None, op0=ALU.is_lt),
                       [slot_f[i].b], [vk[i].b])
                rec.op(V_, lambda e: e.tensor_copy(out=sd_[:, :], in_=slot_f[i][:, :]), [slot_f[i].b], [sd_.b])
                rec.op(V_, lambda e: e.reciprocal(out=res_[i][:, :], in_=res_[i][:, :]), [res_[i].b], [res_[i].b])
                yield
                for k in range(4):
                    rec.op(G_, lambda e, k=k: e.indirect_dma_start(
                        out=xs_d[:, :], out_offset=bass.IndirectOffsetOnAxis(ap=sd_[:, k:k + 1], axis=0),
                        in_=hb[:, :], in_offset=None, bounds_check=rec.bc(e), oob_is_err=False),
                        [sd_.b, hb.b], [dram_xs] if k == 0 else [], dsem=hb.s)
                dram_xs.w = (hb.s.h, hb.s.count, G_, True)
                hb.b.r[hb.s.h] = dram_xs.w
                sd_.b.r[hb.s.h] = dram_xs.w
                rec.op(V_, lambda e: e.tensor_tensor(out=slot_cf[i][:, :], in0=slot_f[i][:, :], in1=vk[i][:, :], op=ALU.mult),
                       [slot_f[i].b, vk[i].b], [slot_cf[i].b])
                rec.op(V_, lambda e: e.tensor_scalar(out=g4[i][:, :], in0=e4[i][:, :], scalar1=res_[i][:, :], scalar2=None,
                                                      op0=ALU.mult), [e4[i].b, res_[i].b], [g4[i].b])
                yield
                rec.op(V_, lambda e: e.tensor_copy(out=slot_c_all[:, Tt, :], in_=slot_cf[i][:, :]),
                       [slot_cf[i].b], [slot_c_all.b])
                rec.op(V_, lambda e: e.tensor_tensor(out=gates_all[:, Tt, :], in0=g4[i][:, :], in1=vk[i][:, :], op=ALU.mult),
                       [g4[i].b, vk[i].b], [gates_all.b])
                yield

            pipeline(route_tile, 32, NB2, 10)
            xs_done = [(h1b[i].s.h, h1b[i].s.count, G_, True) for i in range(4)]
            rec.flush()
            if _STOP == "2a":
                raise _Stop()

        with ExitStack() as s3:
            HS = 512
            pTx = [mk(s3, f"pTx{i}", [128, 1024], BF16, psum=True) for i in range(2)]
            pG = [mk(s3, f"pG{i}", [128, 512], F32, psum=True) for i in range(2)]
            pU = [mk(s3, f"pU{i}", [128, 512], F32, psum=True) for i in range(2)]
            pY = [mk(s3, f"pY{i}", [128, 512], F32, psum=True) for i in range(2)]
            b1 = mk(s3, "b1", [128, NE, 16], F32, dma=True)
            b2bc = [mk(s3, f"b2bc{i}", [128, D], F32, dma=True) for i in range(2)]
            xsb = [mk(s3, f"xsb{i}", [128, 4, D], BF16, dma=True) for i in range(2)]
            xT = [mk(s3, f"xT{i}", [128, 8, HS], BF16) for i in range(2)]
            actT = [mk(s3, f"actT{i}", [128, 8, HS], BF16) for i in range(2)]
            g1 = [mk(s3, f"g1_{i}", [128, 512], F32) for i in range(2)]
            sg = [mk(s3, f"sg_{i}", [128, 512], F32) for i in range(2)]
            u1 = [mk(s3, f"u1_{i}", [128, 512], F32) for i in range(2)]
            tt = [mk(s3, f"tt_{i}", [128, 512], F32) for i in range(2)]
            ysb = [mk(s3, f"ysb{i}", [128, 4, D], BF16, dma=True) for i in range(2)]

            dma(Q_, b1[:, :, :], b1_d[:, :, :], [], [b1.b], b1.s)
            rec.op(V_, lambda e: e.tensor_scalar(out=b1[:, :, 8:16], in0=b1[:, :, 8:16], scalar1=1.0, scalar2=None, op0=ALU.add),
                   [b1.b], [b1.b])

            def load_x(u):
                ex, hf = divmod(u, 2)
                xb = xsb[u % 2]
                for t_ in xs_done:
                    rec.wait_tok(Q_, t_)
                r0 = ex * CAP + hf * HS
                dma(Q_, xb[:, :, :], xs_d[r0:r0 + HS, :].rearrange("(t p) d -> p t d", p=128), [dram_xs], [xb.b], xb.s)
                if hf == 0:
                    dma(Q_, b2bc[ex % 2][:, :], bcast_row(b2_d[ex:ex + 1, :], D), [], [b2bc[ex % 2].b], b2bc[ex % 2].s)

            def transposes(u, dcs=range(8)):
                xb = xsb[u % 2]
                xTe = xT[u % 2]
                for dc in dcs:
                    pt = pTx[dc % 2]
                    rec.op(P_, [lambda e, st_=st_, dc=dc, pt=pt, xb=xb: e.transpose(
                        out=pt[:, st_ * 128:(st_ + 1) * 128], in_=xb[:, st_, dc * 128:(dc + 1) * 128], identity=ident_b[:, :])
                        for st_ in range(4)], [xb.b, ident_b.b], [pt.b])
                    if dc % 2 == 0:
                        rec.op(V_, lambda e, dc=dc, pt=pt, xTe=xTe: e.tensor_copy(out=xTe[:, dc, :], in_=pt[:, 0:HS]),
                               [pt.b], [xTe.b])
                    else:
                        rec.op(A_, lambda e, dc=dc, pt=pt, xTe=xTe: e.activation(out=xTe[:, dc, :], in_=pt[:, 0:HS],
                                                                                 func=AF.Copy), [pt.b], [xTe.b])

            def gemm1(u):
                ex, hf = divmod(u, 2)
                wt = w1[ex % 2]
                xTe = xT[u % 2]
                aT = actT[u % 2]
                for j in range(8):
                    pg, pu = pG[j % 2], pU[j % 2]
                    g1i, sgi, u1i, tti = g1[j % 2], sg[j % 2], u1[j % 2], tt[j % 2]
                    rec.op(P_, [lambda e, dc=dc, j=j, pg=pg: e.matmul(
                        out=pg[:, :], lhsT=wt[:, dc, j * 128:(j + 1) * 128], rhs=xTe[:, dc, :],
                        start=(dc == 0), stop=(dc == 7)) for dc in range(8)], [wt.b, xTe.b], [pg.b])
                    rec.op(P_, [lambda e, dc=dc, j=j, pu=pu: e.matmul(
                        out=pu[:, :], lhsT=wt[:, dc, 1024 + j * 128:1024 + (j + 1) * 128], rhs=xTe[:, dc, :],
                        start=(dc == 0), stop=(dc == 7)) for dc in range(8)], [wt.b, xTe.b], [pu.b])
                    rec.op(V_, lambda e, j=j, pg=pg, g1i=g1i, ex=ex: e.tensor_scalar(
                        out=g1i[:, :], in0=pg[:, :], scalar1=b1[:, ex, j:j + 1], scalar2=7.0,
                        op0=ALU.add, op1=ALU.min), [pg.b, b1.b], [g1i.b])
                    rec.op(A_, lambda e, g1i=g1i, sgi=sgi: e.activation(
                        out=sgi[:, :], in_=g1i[:, :], func=AF.Sigmoid, scale=1.702), [g1i.b], [sgi.b])
                    rec.op(V_, lambda e, j=j, pu=pu, u1i=u1i, ex=ex: e.tensor_scalar(
                        out=u1i[:, :], in0=pu[:, :], scalar1=b1[:, ex, 8 + j:9 + j], scalar2=8.0,
                        op0=ALU.add, op1=ALU.min), [pu.b, b1.b], [u1i.b])
                    rec.op(G_, lambda e, g1i=g1i, sgi=sgi, tti=tti: e.tensor_tensor(
                        out=tti[:, :], in0=g1i[:, :], in1=sgi[:, :], op=ALU.mult), [g1i.b, sgi.b], [tti.b])
                    rec.op(V_, lambda e, j=j, u1i=u1i, tti=tti, aT=aT: e.scalar_tensor_tensor(
                        out=aT[:, j, :], in0=u1i[:, :], scalar=-6.0, in1=tti[:, :],
                        op0=ALU.max, op1=ALU.mult), [u1i.b, tti.b], [aT.b])

            def gemm2(u):
                ex, hf = divmod(u, 2)
                yt = ysb[u % 2]
                bb = b2bc[ex % 2]
                aT = actT[u % 2]
                i = 0
                for st_ in range(4):
                    for nh in range(2):
                        py = pY[i % 2]
                        i += 1
                        rec.op(P_, [lambda e, fc=fc, st_=st_, nh=nh, py=py: e.matmul(
                            out=py[:, :], lhsT=aT[:, fc, st_ * 128:(st_ + 1) * 128], rhs=w2[:, fc, nh * 512:(nh + 1) * 512],
                            start=(fc == 0), stop=(fc == 7)) for fc in range(8)], [aT.b, w2.b], [py.b])
                        rec.op(V_, lambda e, st_=st_, nh=nh, py=py, yt=yt, bb=bb: e.tensor_tensor(
                            out=yt[:, st_, nh * 512:(nh + 1) * 512], in0=py[:, :], in1=bb[:, nh * 512:(nh + 1) * 512],
                            op=ALU.add), [py.b, bb.b], [yt.b])
                r0 = ex * CAP + hf * HS
                dma(Q_, ys_d[r0:r0 + HS, :].rearrange("(t p) d -> p t d", p=128), yt[:, :, :], [yt.b], [], yt.s)

            NU = 2 * NE
            load_x(0)
            transposes(0)
            for u in range(NU):
                ex, hf = divmod(u, 2)
                if hf == 0 and 1 <= ex and ex + 1 < NE:
                    load_w1(ex + 1)
                gemm1(u)
                if u + 1 < NU:
                    load_x(u + 1)
                    transposes(u + 1, range(0, 4))
                gemm2(u)
                if u + 1 < NU:
                    transposes(u + 1, range(4, 8))
                if hf == 1 and ex + 1 < NE:
                    load_w2(ex + 1)
            ys_done = [(ysb[i].s.h, ysb[i].s.count, Q_, True) for i in range(2)]
            rec.flush()
            if _STOP == "2b":
                raise _Stop()

        with ExitStack() as s4:
            NB3 = 4
            ln2_g = mk(s4, "ln2_g", [128, D], F32, dma=True)
            ln2_b = mk(s4, "ln2_b", [128, D], F32, dma=True)
            h1c = [mk(s4, f"h1c{i}", [128, D], F32, dma=True) for i in range(NB3)]
            yg = [mk(s4, f"yg{i}", [128, 4, D], BF16, dma="sw") for i in range(NB3)]
            acc = [mk(s4, f"acc{i}", [128, D], F32) for i in range(NB3)]
            xn2 = [mk(s4, f"xn2{i}", [128, D], F32) for i in range(NB3)]
            ot = [mk(s4, f"ot{i}", [128, D], F32, dma=True) for i in range(NB3)]
            st = [mk(s4, f"st2{i}", [128, 12], F32) for i in range(NB3)]
            mv = [mk(s4, f"mv2{i}", [128, 2], F32) for i in range(NB3)]
            sd = [mk(s4, f"sd2_{i}", [128, 1], F32) for i in range(NB3)]
            rstd = [mk(s4, f"rstd2{i}", [128, 1], F32) for i in range(NB3)]
            nmr = [mk(s4, f"nmr2{i}", [128, 1], F32) for i in range(NB3)]
            dma(Q_, ln2_g[:, :], bcast_row(vecs_d[4:5, :], D), [], [ln2_g.b], ln2_g.s)
            dma(Q_, ln2_b[:, :], bcast_row(vecs_d[5:6, :], D), [], [ln2_b.b], ln2_b.s)
            for t_ in ys_done:
                rec.wait_tok(G_, t_)
            out_toks = []

            def comb_tile(Tt, i):
                hc, ygi, oti, acci = h1c[i], yg[i], ot[i], acc[i]
                dma(Q_, hc[:, :], h1s_d[Tt * 128:(Tt + 1) * 128, :], [], [hc.b], hc.s)
                for k in range(4):
                    rec.op(G_, lambda e, k=k: e.indirect_dma_start(
                        out=ygi[:, k, :], out_offset=None, in_=ys_d[:, :],
                        in_offset=bass.IndirectOffsetOnAxis(ap=slot_c_all[:, Tt, k:k + 1], axis=0),
                        bounds_check=rec.bc(e), oob_is_err=False),
                        [slot_c_all.b], [ygi.b] if k == 0 else [], dsem=ygi.s)
                ygi.b.w = (ygi.s.h, ygi.s.count, G_, True)
                yield
                rec.op(A_, lambda e: e.activation(out=acci[:, :], in_=hc[:, :], func=AF.Copy, scale=ALPHA), [hc.b], [acci.b])
                yield
                for k in range(4):
                    rec.op(V_, lambda e, k=k: e.scalar_tensor_tensor(
                        out=acci[:, :], in0=ygi[:, k, :], scalar=gates_all[:, Tt, k:k + 1], in1=acci[:, :],
                        op0=ALU.mult, op1=ALU.add), [ygi.b, gates_all.b, acci.b], [acci.b])
                    yield
                yield from layernorm_g(acci, 128, ln2_g, ln2_b, oti, xn2[i], st[i], mv[i], sd[i], rstd[i], nmr[i], gmul=G_)
                sq_, tq_ = divmod(Tt, 16)
                out_toks.append(dma(Q_, out_d[sq_, tq_ * 128:(tq_ + 1) * 128, :], oti[:, :], [oti.b], [], oti.s))
                yield

            pipeline(comb_tile, 32, NB3, 4)
            for tk in out_toks[-4:]:
                rec.wait_tok(Q_, tk)
            for e_ in (P_, A_, V_, G_):
                if rec.cnt[e_] > 0:
                    i = rec.cnt[e_] - 1
                    rec.wait_tok(Q_, (rec.esem[e_][i // CH], i % CH + 1, e_, False))
            rec.flush()
            if _STOP == "3":
                raise _Stop()

    except _Stop:
        pass
    return nc


def _rope_tables():
    theta = np.float32(10000.0)

    def cs(pos, dim):
        inv = (theta ** (-(np.arange(0, dim, 2, dtype=np.float32) / np.float32(dim)))).astype(np.float32)
        ang = (pos.astype(np.float32)[:, None] * inv[None, :]).astype(np.float32)
        return np.cos(ang.astype(np.float64)).astype(np.float32), np.sin(ang.astype(np.float64)).astype(np.float32)

    pos1 = np.concatenate([np.arange(NMETA, L), np.arange(NMETA)])
    row = np.concatenate([np.arange(SEQ) // 64, np.full(NMETA, -1)])
    col = np.concatenate([np.arange(SEQ) % 64, np.arange(NMETA)])
    c1, s1 = cs(pos1, 64)
    cr, sr = cs(row, 64)
    cc, sc = cs(col, 64)
    return np.concatenate([c1, c1, s1, s1, cr, cr, cc, cc, sr, sr, sc, sc], axis=1).astype(np.float32)


_NC_CACHE = {}


def kernel(x, meta_tokens, ln_emb_g, ln_emb_b, w_in, g_q_a, w_q_b, g_kv_a, w_kv_b,
           g_q_gqa, g_k_gqa, g_o_mla, g_o_gqa, w_o, ln1_g, ln1_b,
           w_router, b_router, w_gate_up, b_gate_up, w_down, b_down, ln2_g, ln2_b):
    f = lambda a: np.ascontiguousarray(np.asarray(a, dtype=np.float32))
    x = f(x)
    vecs = np.zeros((8, D), np.float32)
    vecs[0], vecs[1] = f(ln_emb_g), f(ln_emb_b)
    vecs[2], vecs[3] = f(ln1_g)[0], f(ln1_b)[0]
    vecs[4], vecs[5] = f(ln2_g)[0], f(ln2_b)[0]
    vecs[6, :512], vecs[6, 512:] = f(g_o_mla)[0], f(g_o_gqa)[0]
    gsm = np.stack([f(g_q_gqa)[0], f(g_k_gqa)[0]])
    gpp = np.zeros((128, 4), np.float32)
    gpp[:, 0], gpp[:, 1] = f(g_q_a)[0, :128], f(g_q_a)[0, 128:]
    gpp[:, 2] = f(g_kv_a)[0]
    wgu = f(w_gate_up)[0]
    w1 = np.ascontiguousarray(np.concatenate([wgu[:, :, 0::2], wgu[:, :, 1::2]], axis=2))
    bgu = f(b_gate_up)[0]
    b1 = np.concatenate([bgu[:, 0::2], bgu[:, 1::2]], axis=1)
    b1t = np.ascontiguousarray(b1.reshape(NE, 16, 128).transpose(2, 0, 1))
    cst = np.zeros((128, 448), np.float32)
    cst[:, 0:128] = np.eye(128, dtype=np.float32)
    cst[:, 128:256] = np.triu(np.ones((128, 128), np.float32), 1)
    cst[:, 256:384] = 1.0
    cst[:, 384:416] = (np.arange(NE, dtype=np.float32) * CAP)[None, :]
    cst[:, 416:448] = np.arange(NE, dtype=np.float32)[None, :]
    shared = {
        "meta": f(meta_tokens), "vecs": vecs, "gsm": gsm, "gpp": gpp,
        "w_in": f(w_in)[0], "w_qb": f(w_q_b)[0], "w_kvb": f(w_kv_b)[0], "w_o": f(w_o)[0],
        "w_r": f(w_router)[0], "b_r": f(b_router), "w1": w1 if not _SMALL else w1[:1], "b1t": b1t,
        "w2": f(w_down)[0] if not _SMALL else f(w_down)[0][:1],
        "b2": f(b_down)[0], "rope": _rope_tables(), "cst": cst,
    }
    if "nc" not in _NC_CACHE:
        _NC_CACHE["nc"] = build_nc()
    nc = _NC_CACHE["nc"]
    in_maps = []
    for c in range(NCORES):
        m = dict(shared)
        m["x"] = np.ascontiguousarray(x[2 * c:2 * c + 2])
        in_maps.append(m)
    res = run_bass_kernel_spmd(nc, in_maps, core_ids=list(range(NCORES)))
    return np.concatenate([np.asarray(r["out"], dtype=np.float32) for r in res.results], axis=0)
```
